# Optimizing a Trainium2 kernel written in Bass

```python
import jax, jax.numpy as jnp
from jax import lax
import numpy as np

D_MODEL = 1024
BATCH = 8
SEQ = 4096
DEPTH = 1

N_HEADS = 16
N_KV_HEADS = 4
HEAD_DIM = 64
Q_PER_KV = N_HEADS // N_KV_HEADS
WINDOW = 128
BLOCK = 128
ATTN_WIDTH = N_HEADS * HEAD_DIM
KV_WIDTH = N_KV_HEADS * HEAD_DIM
GMLP_WIDTH = 1024
GMLP_GROUPS = 8
GMLP_GROUP_DIM = GMLP_WIDTH // GMLP_GROUPS
GMLP_CHUNK = 128
N_EXPERT_GROUPS = 4
EXPERTS_PER_GROUP = 4
N_EXPERTS = N_EXPERT_GROUPS * EXPERTS_PER_GROUP
TOP_K = 2
D_EXPERT = 512

N_BRANCHES = 2
IN_WIDTH = ATTN_WIDTH + 2 * KV_WIDTH + 2 * GMLP_WIDTH + N_BRANCHES * D_MODEL
SPLITS = (ATTN_WIDTH,
          ATTN_WIDTH + KV_WIDTH,
          ATTN_WIDTH + 2 * KV_WIDTH,
          ATTN_WIDTH + 2 * KV_WIDTH + GMLP_WIDTH,
          ATTN_WIDTH + 2 * KV_WIDTH + 2 * GMLP_WIDTH)
EPS = 1e-6
NEG_INF = -1e30

kernel_name = "hybrid_swa_gmlp_hmoe_adaln"


def alibi_slopes(n_heads):
    return np.array([2.0 ** (-8.0 * (i + 1) / n_heads) for i in range(n_heads)], dtype=np.float32)


def rmsnorm(x, gain):
    xf = x.astype(jnp.float32)
    y = xf * lax.rsqrt(jnp.mean(xf * xf, axis=-1, keepdims=True) + EPS)
    return y.astype(x.dtype) * gain


def layernorm(x, gain, bias):
    xf = x.astype(jnp.float32)
    mu = jnp.mean(xf, axis=-1, keepdims=True)
    var = jnp.mean(jnp.square(xf - mu), axis=-1, keepdims=True)
    y = (xf - mu) * lax.rsqrt(var + EPS)
    return y.astype(x.dtype) * gain + bias


def modulate(h, shift, scale):
    return h * (1.0 + scale[:, None, :]) + shift[:, None, :]


def sliding_window_attention(q, k, v, q_gain, k_gain, sinks):
    B, S = q.shape[0], q.shape[1]
    nb = S // BLOCK
    q = rmsnorm(q, q_gain)
    k = rmsnorm(k, k_gain)
    qb = q.reshape(B, nb, BLOCK, N_KV_HEADS, Q_PER_KV, HEAD_DIM)
    kb = k.reshape(B, nb, BLOCK, N_KV_HEADS, HEAD_DIM)
    vb = v.reshape(B, nb, BLOCK, N_KV_HEADS, HEAD_DIM)
    pad = ((0, 0), (1, 0), (0, 0), (0, 0), (0, 0))
    k_cat = jnp.concatenate([jnp.pad(kb, pad)[:, :-1], kb], axis=2)
    v_cat = jnp.concatenate([jnp.pad(vb, pad)[:, :-1], vb], axis=2)
    scores = jnp.einsum('bnqkgd,bnskd->bnkgqs', qb, k_cat).astype(jnp.float32) * (HEAD_DIM ** -0.5)
    q_idx = jnp.arange(BLOCK)[:, None] + BLOCK
    k_idx = jnp.arange(2 * BLOCK)[None, :]
    dist = q_idx - k_idx
    in_window = (dist >= 0) & (dist < WINDOW)
    blk = jnp.arange(nb)[:, None, None]
    valid = in_window[None] & ((blk > 0) | (k_idx >= BLOCK)[None])
    slopes = jnp.asarray(alibi_slopes(N_HEADS)).reshape(N_KV_HEADS, Q_PER_KV)
    scores = scores - slopes[None, None, :, :, None, None] * dist.astype(jnp.float32)[None, None, None, None]
    scores = jnp.where(valid[None, :, None, None], scores, NEG_INF)
    sink = sinks.astype(jnp.float32).reshape(N_KV_HEADS, Q_PER_KV)[None, None, :, :, None, None]
    sink = jnp.broadcast_to(sink, scores.shape[:-1] + (1,))
    probs = jax.nn.softmax(jnp.concatenate([scores, sink], axis=-1), axis=-1)[..., :-1]
    out = jnp.einsum('bnkgqs,bnskd->bnqkgd', probs.astype(v_cat.dtype), v_cat)
    return out.reshape(B, S, ATTN_WIDTH)


def chunked_spatial_gating(u, vg, norm_gain, norm_bias, w_spatial, b_spatial):
    B, S = u.shape[0], u.shape[1]
    nc = S // GMLP_CHUNK
    vg = layernorm(vg, norm_gain, norm_bias)
    vc = vg.reshape(B, nc, GMLP_CHUNK, GMLP_GROUPS, GMLP_GROUP_DIM)
    tril = jnp.tril(jnp.ones((GMLP_CHUNK, GMLP_CHUNK), dtype=bool))
    w = jnp.where(tril[None], w_spatial, 0.0)
    mixed = jnp.einsum('gts,bnsgc->bntgc', w, vc) + b_spatial.T[None, None, :, :, None]
    return u * mixed.reshape(B, S, GMLP_WIDTH)


def hierarchical_moe(h, w_group_router, b_group_router, w_expert_router, b_expert_router, w_gate, w_up, w_down):
    B, S, D = h.shape
    hf = h.reshape(B * S, D)
    g_logits = (hf @ w_group_router).astype(jnp.float32) + b_group_router
    g_prob = jax.nn.softmax(g_logits, axis=-1)
    g_idx = jnp.argmax(g_logits, axis=-1)
    g_weight = jnp.take_along_axis(g_prob, g_idx[:, None], axis=-1)
    e_logits = ((hf @ w_expert_router).astype(jnp.float32) + b_expert_router).reshape(-1, N_EXPERT_GROUPS, EXPERTS_PER_GROUP)
    e_in_group = jnp.take_along_axis(e_logits, g_idx[:, None, None], axis=1)[:, 0]
    top_vals, top_idx = lax.top_k(e_in_group, TOP_K)
    top_w = jax.nn.softmax(top_vals, axis=-1) * g_weight
    w_in_group = jnp.sum(jax.nn.one_hot(top_idx, EXPERTS_PER_GROUP, dtype=jnp.float32) * top_w[..., None], axis=1)
    combine = jax.nn.one_hot(g_idx, N_EXPERT_GROUPS, dtype=jnp.float32)[:, :, None] * w_in_group[:, None, :]
    combine = combine.astype(hf.dtype)
    out = jnp.zeros_like(hf)
    for gi in range(N_EXPERT_GROUPS):
        a = jnp.einsum('td,edf->tef', hf, w_gate[gi])
        b = jnp.einsum('td,edf->tef', hf, w_up[gi])
        hid = jax.nn.silu(a) * b * combine[:, gi, :, None]
        out = out + jnp.einsum('tef,efd->td', hid, w_down[gi])
    return out.reshape(B, S, D)


def setup_inputs(seed: int = 0) -> dict:
    key = jax.random.key(seed)
    ks = jax.random.split(key, 25)
    L, D = DEPTH, D_MODEL

    def nrm(k, shape, s):
        return jax.random.normal(k, shape, jnp.float32) * s

    return {
        "x": nrm(ks[0], (BATCH, SEQ, D), 1.0),
        "c": nrm(ks[1], (BATCH, D), 1.0),
        "w_ada": nrm(ks[2], (L, D, 6 * D), 0.2 * D ** -0.5),
        "b_ada": nrm(ks[3], (L, 6 * D), 0.02),
        "norm1_gain": 1.0 + nrm(ks[4], (L, D), 0.02),
        "w_in": nrm(ks[5], (L, D, IN_WIDTH), D ** -0.5),
        "b_branch_gate": nrm(ks[6], (L, N_BRANCHES * D), 0.02),
        "q_norm_gain": 1.0 + nrm(ks[7], (L, HEAD_DIM), 0.02),
        "k_norm_gain": 1.0 + nrm(ks[8], (L, HEAD_DIM), 0.02),
        "attn_sinks": nrm(ks[9], (L, N_HEADS), 0.5),
        "gmlp_norm_gain": 1.0 + nrm(ks[10], (L, GMLP_WIDTH), 0.02),
        "gmlp_norm_bias": nrm(ks[11], (L, GMLP_WIDTH), 0.02),
        "gmlp_w_spatial": nrm(ks[12], (L, GMLP_GROUPS, GMLP_CHUNK, GMLP_CHUNK), 0.5 * GMLP_CHUNK ** -0.5),
        "gmlp_b_spatial": 1.0 + nrm(ks[13], (L, GMLP_GROUPS, GMLP_CHUNK), 0.02),
        "w_o_attn": nrm(ks[14], (L, ATTN_WIDTH, D), ATTN_WIDTH ** -0.5),
        "w_o_gmlp": nrm(ks[15], (L, GMLP_WIDTH, D), GMLP_WIDTH ** -0.5),
        "w_out": nrm(ks[16], (L, D, D), D ** -0.5),
        "norm2_gain": 1.0 + nrm(ks[17], (L, D), 0.02),
        "w_group_router": nrm(ks[18], (L, D, N_EXPERT_GROUPS), D ** -0.5),
        "b_group_router": nrm(ks[19], (L, N_EXPERT_GROUPS), 0.01),
        "w_expert_router": nrm(ks[20], (L, D, N_EXPERTS), D ** -0.5),
        "b_expert_router": nrm(ks[21], (L, N_EXPERTS), 0.01),
        "w_expert_gate": nrm(ks[22], (L, N_EXPERT_GROUPS, EXPERTS_PER_GROUP, D, D_EXPERT), D ** -0.5),
        "w_expert_up": nrm(ks[23], (L, N_EXPERT_GROUPS, EXPERTS_PER_GROUP, D, D_EXPERT), D ** -0.5),
        "w_expert_down": nrm(ks[24], (L, N_EXPERT_GROUPS, EXPERTS_PER_GROUP, D_EXPERT, D), D_EXPERT ** -0.5),
    }


def reference(x, c, w_ada, b_ada, norm1_gain, w_in, b_branch_gate, q_norm_gain, k_norm_gain, attn_sinks,
              gmlp_norm_gain, gmlp_norm_bias, gmlp_w_spatial, gmlp_b_spatial, w_o_attn, w_o_gmlp, w_out,
              norm2_gain, w_group_router, b_group_router, w_expert_router, b_expert_router,
              w_expert_gate, w_expert_up, w_expert_down):
    B, S, _ = x.shape
    c_act = jax.nn.silu(c)
    for l in range(DEPTH):
        mod = c_act @ w_ada[l] + b_ada[l]
        shift1, scale1, gate1, shift2, scale2, gate2 = jnp.split(mod, 6, axis=-1)

        h = modulate(rmsnorm(x, norm1_gain[l]), shift1, scale1)
        proj = h @ w_in[l]
        q, k, v, u, vg, gate_logits = jnp.split(proj, SPLITS, axis=-1)
        q = q.reshape(B, S, N_HEADS, HEAD_DIM)
        k = k.reshape(B, S, N_KV_HEADS, HEAD_DIM)
        v = v.reshape(B, S, N_KV_HEADS, HEAD_DIM)
        y_attn = sliding_window_attention(q, k, v, q_norm_gain[l], k_norm_gain[l], attn_sinks[l])
        u = jax.nn.gelu(u, approximate=False)
        vg = jax.nn.gelu(vg, approximate=False)
        y_gmlp = chunked_spatial_gating(u, vg, gmlp_norm_gain[l], gmlp_norm_bias[l], gmlp_w_spatial[l], gmlp_b_spatial[l])
        gates = jax.nn.sigmoid(gate_logits + b_branch_gate[l])
        g_attn, g_gmlp = jnp.split(gates, N_BRANCHES, axis=-1)
        merged = g_attn * (y_attn @ w_o_attn[l]) + g_gmlp * (y_gmlp @ w_o_gmlp[l])
        x = x + gate1[:, None, :] * (merged @ w_out[l])

        h2 = modulate(rmsnorm(x, norm2_gain[l]), shift2, scale2)
        y_moe = hierarchical_moe(h2, w_group_router[l], b_group_router[l], w_expert_router[l], b_expert_router[l],
                                 w_expert_gate[l], w_expert_up[l], w_expert_down[l])
        x = x + gate2[:, None, :] * y_moe
    return x
```

```python
import numpy as np
from contextlib import ExitStack
import concourse.bass as bass
import concourse.mybir as mybir
from concourse.bass_utils import run_bass_kernel_spmd

F32 = mybir.dt.float32
BF16 = mybir.dt.bfloat16
AF = mybir.ActivationFunctionType
ALU = mybir.AluOpType
AX = mybir.AxisListType

S = 4096
D = 1024
STT = 1024
NBLK = 8
NSLOT = 6
EPS = 1e-6
NOHOIST = False
HOIST_EX = 11
ENGS = ("pe", "act", "dve", "pool", "sp")


class Region:
    __slots__ = ("name", "last_w", "extra_w", "readers")

    def __init__(self, name):
        self.name = name
        self.last_w = None
        self.extra_w = []
        self.readers = {}


class Sync:
    def __init__(self, n_dma_sp=8, n_dma_pool=6):
        self.prog = {e: [] for e in ENGS}
        self.cnt = {e: 0 for e in ENGS}
        self.waited = {e: {} for e in ENGS}
        self.dma_sems = {"sp": [f"dsp{i}" for i in range(n_dma_sp)],
                         "pool": [f"dpl{i}" for i in range(n_dma_pool)]}
        self.dma_tot = {}
        for q in self.dma_sems:
            for k in self.dma_sems[q]:
                self.dma_tot[k] = 0
        self.dma_rr = {"sp": 0, "pool": 0}
        self.n_ops = 0

    def sem_names(self):
        return list(ENGS) + [k for q in self.dma_sems for k in self.dma_sems[q]]

    def _wait(self, e, ev):
        k, v = ev
        if self.waited[e].get(k, 0) >= v:
            return
        self.waited[e][k] = v
        self.prog[e].append(("wait", k, v))

    def _deps(self, e, reads, writes, parallel=False):
        deps = []
        for r in reads:
            if r.last_w is not None:
                deps.append(r.last_w)
            deps.extend(r.extra_w)
        for w in writes:
            if not parallel:
                if w.last_w is not None:
                    deps.append(w.last_w)
                deps.extend(w.extra_w)
            deps.extend(w.readers.items())
        for ev in deps:
            if ev[0] == e and e == "pe":
                continue
            self._wait(e, ev)

    def _commit(self, ev, reads, writes, parallel=False):
        k, v = ev
        for r in reads:
            if r.readers.get(k, 0) < v:
                r.readers[k] = v
        for w in writes:
            if parallel:
                w.extra_w.append(ev)
            else:
                w.last_w = ev
                w.extra_w = []
            w.readers = {}

    def op(self, e, fn, reads=(), writes=(), inc=True):
        self._deps(e, reads, writes)
        ev = (e, self.cnt[e] + 1)
        if inc:
            self.cnt[e] += 1
            self.prog[e].append(("op", _capture(fn), e, 1))
        else:
            self.prog[e].append(("op", _capture(fn), None, 0))
        self._commit(ev, reads, writes)
        self.n_ops += 1

    def dma(self, q, fn, reads=(), writes=(), parallel=False):
        sems = self.dma_sems[q]
        k = sems[self.dma_rr[q] % len(sems)]
        self.dma_rr[q] += 1
        if self.dma_tot[k] > 0:
            self._wait(q, (k, self.dma_tot[k]))
        self._deps(q, reads, writes, parallel)
        self.dma_tot[k] += 16
        ev = (k, self.dma_tot[k])
        self.prog[q].append(("op", _capture(fn), k, 16))
        self._commit(ev, reads, writes, parallel)
        return ev

    def finish(self, e="sp"):
        for k, v in self.dma_tot.items():
            if v > 0:
                self._wait(e, (k, v))
        for k in ENGS:
            if k != e and self.cnt[k] > 0:
                self._wait(e, (k, self.cnt[k]))

    def replay(self, nc, block, sems):
        handles = {"pe": "tensor", "act": "scalar", "dve": "vector", "pool": "gpsimd", "sp": "sync"}

        def make(e):
            def body(eng):
                for item in self.prog[e]:
                    if item[0] == "wait":
                        eng.wait_ge(sems[item[1]], item[2])
                    else:
                        name, a, k = item[1]
                        ins = getattr(eng, name)(*a, **k)
                        if item[2] is not None:
                            ins.then_inc(sems[item[2]], item[3])
            return body

        for e in ENGS:
            getattr(block, handles[e])(make(e))


class _Rec:
    def __init__(self):
        self.call = None

    def __getattr__(self, name):
        def f(*a, **k):
            self.call = (name, a, k)
            return self
        return f


def _capture(fn):
    r = _Rec()
    fn(r)
    assert r.call is not None
    return r.call


class RR:
    def __init__(self, tiles, name):
        self.tiles = tiles
        self.regs = [Region(f"{name}{i}") for i in range(len(tiles))]
        self.i = 0

    def next(self):
        j = self.i % len(self.tiles)
        self.i += 1
        return self.tiles[j], self.regs[j]


def alibi_tables():
    slopes = np.array([2.0 ** (-8.0 * (i + 1) / 16) for i in range(16)], dtype=np.float64)
    s_idx = np.arange(128)[:, None]
    q_idx = np.arange(128)[None, :]
    E = np.zeros((128, 4, 2, 2, 2, 128), dtype=np.float32)
    for g in range(4):
        for i in range(2):
            for cc in range(2):
                h = 4 * g + 2 * cc + i
                d1 = (q_idx - s_idx).astype(np.float64)
                E[:, g, i, 1, cc, :] = np.where(d1 >= 0, np.exp(-slopes[h] * d1), 0.0)
                d0 = (q_idx + 128 - s_idx).astype(np.float64)
                E[:, g, i, 0, cc, :] = np.where(d0 < 128, np.exp(-slopes[h] * d0), 0.0)
    return E.reshape(128, 4096)


class _Stop(Exception):
    pass


def build_nc(n_st=4, dbg=False, upto=None):
    nc = bass.Bass("TRN2", target_bir_lowering=False)

    def din(name, shape):
        return nc.dram_tensor(name, list(shape), F32, kind="ExternalInput").ap()

    x = din("x", [S, D])
    c_fm = din("c_fm", [128, 8])
    w_ada = din("w_ada", [D, 6 * D])
    b_ada_fm = din("b_ada_fm", [128, 48])
    b_ada_row = din("b_ada_row", [1, 6 * D])
    n1_fm = din("n1_fm", [128, 8])
    n2_fm = din("n2_fm", [128, 8])
    w_in = din("w_in", [D, 5632])
    bgate_fm = din("bgate_fm", [128, 16])
    qg_fm = din("qg_fm", [128, 1])
    kg_fm = din("kg_fm", [128, 1])
    sinks_row = din("sinks_row", [1, 16])
    gg_fm = din("gg_fm", [128, 8])
    gb_rows = din("gb_rows", [8, 128])
    wsT = din("wsT", [128, 1024])
    bsp_row = din("bsp_row", [1, 1024])
    w_oa = din("w_oa", [D, D])
    w_og = din("w_og", [D, D])
    w_out = din("w_out", [D, D])
    wr = din("wr", [D, 20])
    br_row = din("br_row", [1, 20])
    w_eg = din("w_eg", [16, D, 512])
    w_eu = din("w_eu", [16, D, 512])
    w_ed = din("w_ed", [16, 512, D])
    k_ident = din("k_ident", [128, 128])
    k_etab = din("k_etab", [128, 4096])
    k_maskT = din("k_maskT", [128, 128])
    k_bones = din("k_bones", [128, 128])
    k_sel = din("k_sel", [16, 2048])
    k_ones = din("k_ones", [1, 128])
    out = nc.dram_tensor("out", [S, D], F32, kind="ExternalOutput").ap()

    K = Sync()
    es = ExitStack()

    def sb(name, shape, dt):
        return es.enter_context(nc.sbuf_tensor(name, list(shape), dt))

    BB = sb("BB", [128, 16384], BF16)
    B1 = BB[:, 0:8192].rearrange("p (k t) -> p k t", k=8)
    B2 = BB[:, 8192:16384].rearrange("p (k t) -> p k t", k=8)
    acc = BB[:].bitcast(F32).rearrange("p (b d) -> p b d", b=8)
    B3 = sb("B3", [128, 8, 1024], BF16)
    hT = sb("hT", [128, 8, 1024], BF16)
    slots = [sb(f"slot{i}", [128, 4096], BF16) for i in range(NSLOT)]
    kT = sb("kT", [128, 4, 1152], BF16)
    vaug = sb("vaug", [128, 9, 4, 72], BF16)
    etab = sb("etab", [128, 4096], BF16)
    R2 = sb("R2", [128, 8, 128], F32)
    g1b = sb("g1b", [128, 1024], BF16)
    g2b = sb("g2b", [128, 1024], BF16)
    WmT = sb("WmT", [128, 8, 128], BF16)
    ident_f = sb("ident_f", [128, 128], F32)
    ident_b = sb("ident_b", [128, 128], BF16)
    bones = sb("bones", [128, 128], BF16)
    selb = sb("selb", [16, 16, 128], BF16)
    wr_sb = sb("wr_sb", [128, 8, 20], F32)
    br_b = sb("br_b", [128, 20], F32)
    esink = sb("esink", [128, 16], F32)
    combT = sb("combT", [16, 1024], BF16)
    cst = sb("cst", [128, 96], F32)
    modfm = sb("modfm", [128, 48], F32)
    bfm = sb("bfm", [128, 48], F32)
    c_bf = sb("c_bf", [128, 8], BF16)
    c_rep = sb("c_rep", [128, 8, 128], BF16)
    ones_bf = sb("ones_bf", [128, 1], BF16)
    small = sb("small", [128, 128], F32)
    rt_tile = sb("rt_tile", [128, 816], F32)
    r_rt = Region("rt")
    A1 = cst[:, 0:8]; S1 = cst[:, 8:16]; A2 = cst[:, 16:24]; S2 = cst[:, 24:32]
    N1 = cst[:, 32:40]; N2 = cst[:, 40:48]; QG = cst[:, 48:49]; KG = cst[:, 49:50]
    GG = cst[:, 50:58]; BG = cst[:, 58:74]; CF = cst[:, 74:82]; CA = cst[:, 82:90]

    f32a = RR([sb(f"f32a{i}", [128, 1024], F32) for i in range(3)], "f32a")
    f32h = RR([sb(f"f32h{i}", [128, 512], F32) for i in range(5)], "f32h")
    bfh = RR([sb(f"bfh{i}", [128, 512], BF16) for i in range(6)], "bfh")
    ptp = RR([sb(f"pt{i}", [128, 512], BF16) for i in range(6)], "pt")
    xsb = RR([sb(f"xsb{i}", [128, 1024], BF16) for i in range(2)], "xsb")
    ytm = RR([sb(f"ytm{i}", [128, 1024], BF16) for i in range(1)], "ytm")
    hidp = RR([sb(f"hid{i}", [128, 4, 512], BF16) for i in range(2)], "hid")

    PS = es.enter_context(nc.psum_tensor("PS", [128, 4096], F32))
    bank_r = [Region(f"bank{i}") for i in range(8)]
    pcur = [0]

    def nb(n=1):
        c = pcur[0]
        if c % n:
            c += n - (c % n)
        c %= 8
        pcur[0] = c + n
        return c, bank_r[c:c + n]

    hcur = [0]

    def nbhi():
        c = 2 + hcur[0] % 6
        hcur[0] += 1
        return c, bank_r[c:c + 1]

    def bank(i, w=512, off=0):
        return PS[:, i * 512 + off: i * 512 + off + w]

    def bank_bf(i):
        return PS[:, i * 512:(i + 1) * 512].bitcast(BF16)

    r_B1 = Region("B1"); r_B2 = Region("B2"); r_B3 = Region("B3")
    r_hT = [Region(f"hT{i}") for i in range(NBLK)]
    r_acc = [Region(f"acc{i}") for i in range(NBLK)]
    r_slot = [Region(f"slot{i}") for i in range(NSLOT)]
    r_kT = Region("kT"); r_v = Region("vaug")
    r_const = Region("const")
    r_small = Region("small"); r_comb = Region("combT")
    r_sm = [Region(f"sm{i}") for i in range(32)]
    W_B1 = [r_B1] + r_acc[0:4]
    W_B2 = [r_B2] + r_acc[4:8]

    scur = [0]

    def load_unit(src_ap, width=4096, view=None):
        i = scur[0] % NSLOT
        scur[0] += 1
        dst = s8(slots[i]) if view is None else view(slots[i])
        K.dma("pool", lambda e, d=dst, s=src_ap: e.dma_start(out=d, in_=s), reads=[], writes=[r_slot[i]])
        return slots[i], r_slot[i]

    def wview(W, c0, w):
        return W[:, c0:c0 + w].rearrange("(kc p) f -> p kc f", p=128)

    def s8(slot, w=512):
        return slot[:, 0:8 * w].rearrange("p (k f) -> p k f", k=8)

    def dump(name, ap2d, regs, dt):
        if not dbg:
            return
        t = nc.dram_tensor("dbg_" + name, list(ap2d.shape), dt, kind="ExternalOutput").ap()
        K.dma("sp", lambda e: e.dma_start(out=t[:, :], in_=ap2d), reads=list(regs), writes=[])

    def ld(dst, src, q="sp", regs=(r_const,)):
        K.dma(q, lambda e, d=dst, s=src: e.dma_start(out=d, in_=s), reads=[], writes=list(regs), parallel=True)

    r_c0 = Region("c_in")
    ld(CF, c_fm[:, :], regs=(r_c0,))
    ld(bfm[:, 0:48], b_ada_fm[:, :], regs=(r_const,))
    ld(N1, n1_fm[:, :], regs=(r_const,))
    ld(ident_b[:], k_ident[:, :], q="pool", regs=(r_const,))
    scur[0] = 0
    i_first = scur[0] % NSLOT
    K.dma("pool", lambda e: e.dma_start(out=s8(slots[i_first]), in_=wview(w_ada, 0, 512)), reads=[r_c0, r_const], writes=[r_slot[i_first]])
    scur[0] += 1
    ada_pre = [(slots[i_first], r_slot[i_first])] + [load_unit(wview(w_ada, 512 * u, 512)) for u in range(1, 4)]
    ld(N2, n2_fm[:, :]); ld(QG, qg_fm[:, :]); ld(KG, kg_fm[:, :])
    ld(GG, gg_fm[:, :]); ld(BG, bgate_fm[:, :])
    ld(ident_f[:], k_ident[:, :])
    ld(wr_sb[:], wr.rearrange("(kc p) j -> p kc j", p=128))
    ld(br_b[:], br_row.partition_broadcast(128))
    ld(esink[:], sinks_row.partition_broadcast(128))
    wsf, wsf_r = f32a.next()
    r2l_t, r2l_r = f32a.next()
    r2r_t, r2r_r = f32a.next()
    r2l = r2l_t[0:2, :].rearrange("p (g t) -> p g t", g=8)
    r2r = r2r_t[0:2, :]
    ld(r2l[0:1, :, :], gb_rows.rearrange("(o g) t -> o g t", o=1), regs=(r2l_r,))
    for g in range(8):
        ld(r2l[1:2, g, :], k_ones[0:1, :], regs=(r2l_r,))
    ld(r2r[1:2, :], bsp_row[0:1, :], regs=(r2r_r,))
    ld(wsf[:], wsT[:, :], regs=(wsf_r,))
    mkt, mkt_r = f32h.next()
    ld(mkt[:, 0:128], k_maskT[:, :], regs=(mkt_r,))
    ld(bones[:], k_bones[:, :], q="pool")
    ld(ones_bf[:], k_ones[0:1, :].rearrange("o p -> p o"), q="pool")
    ld(etab[:, 0:2048], k_etab[:, 0:2048], q="pool")
    ld(etab[:, 2048:4096], k_etab[:, 2048:4096], q="pool")
    ld(selb[:].rearrange("k e m -> k (e m)"), k_sel[:, :], q="pool")

    r_c = Region("c_act")
    K.op("act", lambda e: e.activation(out=CA, in_=CF, func=AF.Silu), reads=[r_c0], writes=[r_c0])
    K.op("dve", lambda e: e.tensor_copy(out=c_bf[:], in_=CA), reads=[r_c0], writes=[r_c])
    K.op("dve", lambda e: e.tensor_copy(out=c_rep[:], in_=CA.unsqueeze(2).to_broadcast([128, 8, 128])),
         reads=[r_c0], writes=[r_c])
    K.op("act", lambda e: e.activation(out=esink[:], in_=esink[:], func=AF.Exp), reads=[r_const], writes=[r_const])
    K.op("dve", lambda e: e.tensor_tensor(out=WmT[:], in0=wsf[:].rearrange("p (g t) -> p g t", g=8),
                                          in1=mkt[:, 0:128].unsqueeze(1).to_broadcast([128, 8, 128]), op=ALU.mult),
         reads=[wsf_r, mkt_r], writes=[r_const])
    K.op("dve", lambda e: e.memset(vaug[:], 1.0), reads=[], writes=[r_v])
    K.op("dve", lambda e: e.memset(small[:, 120:121], -0.5), reads=[], writes=[r_const])
    K.op("dve", lambda e: e.memset(kT[:], 0.0), reads=[], writes=[r_kT])

    b0, br_ = nb(2)
    for hh in range(2):
        K.op("pe", lambda e, hh=hh: e.matmul(PS[0:1, (b0 + hh) * 512:(b0 + hh + 1) * 512], ones_bf[:, 0:1],
                                            WmT[:, 4 * hh:4 * hh + 4, :], start=True, stop=True),
             reads=[r_const], writes=[br_[hh]])
    K.op("act", lambda e: e.activation(out=r2r[0:1, :], in_=PS[0:1, b0 * 512:(b0 + 2) * 512], func=AF.Copy),
         reads=br_, writes=[r2r_r])
    b0, br_ = nb(2)
    for g in range(8):
        K.op("pe", lambda e, g=g: e.matmul(PS[:, b0 * 512 + g * 128: b0 * 512 + (g + 1) * 128], r2l[0:2, g, :],
                                          r2r[0:2, g * 128:(g + 1) * 128], start=True, stop=True),
             reads=[r_const, r2l_r, r2r_r], writes=[br_[g // 4]], inc=(g % 4 == 3))
    K.op("act", lambda e: e.activation(out=R2[:].rearrange("p g t -> p (g t)"), in_=PS[:, b0 * 512:(b0 + 2) * 512],
                                       func=AF.Copy), reads=br_, writes=[r_const])

    r_g1 = Region("g1b"); r_a2 = Region("a2s2"); r_g2 = Region("g2b"); r_mod = Region("modfm")

    def ada_fm(units, lo_, hi_, pre=None):
        mb, mbr = nb(1)
        for n_, u in enumerate(units):
            slot, sr = pre[n_] if pre is not None else load_unit(wview(w_ada, 512 * u, 512))
            w8 = s8(slot)
            for fl in range(4):
                j = 4 * u + fl - lo_
                for kc in range(8):
                    K.op("pe", lambda e, kc=kc: e.matmul(
                        bank(mb, 1, j), w8[:, kc, fl * 128:(fl + 1) * 128], c_bf[:, kc:kc + 1],
                        start=(kc == 0), stop=(kc == 7)),
                         reads=[sr, r_c], writes=mbr, inc=(kc == 7 and fl == 3))
        K.op("dve", lambda e: e.tensor_tensor(out=modfm[:, lo_:hi_], in0=bank(mb, hi_ - lo_), in1=bfm[:, lo_:hi_], op=ALU.add),
             reads=mbr + [r_const], writes=[r_mod])

    def ada_rows(units, dst, dst_r):
        for n_, u in enumerate(units):
            slot, sr = load_unit(wview(w_ada, 512 * u, 512))
            w8 = s8(slot)
            gb, gbr = nb(1)
            for kc in range(8):
                K.op("pe", lambda e, kc=kc: e.matmul(bank(gb), c_rep[:, kc, :], w8[:, kc, :], start=(kc == 0), stop=(kc == 7)),
                     reads=[sr, r_c], writes=gbr, inc=(kc == 7))
            bt, bt_r = f32h.next()
            ld(bt[:], b_ada_row[0:1, 512 * u:512 * (u + 1)].partition_broadcast(128), regs=(bt_r,))
            K.op("dve", lambda e: e.tensor_tensor(out=dst[:, 512 * n_:512 * (n_ + 1)], in0=bank(gb), in1=bt[:], op=ALU.add),
                 reads=gbr + [bt_r], writes=[dst_r])

    ada_fm([0, 1, 2, 3], 0, 16, pre=ada_pre)
    K.op("dve", lambda e: e.scalar_tensor_tensor(out=A1, in0=modfm[:, 8:16], scalar=1.0, in1=N1, op0=ALU.add, op1=ALU.mult),
         reads=[r_const, r_mod], writes=[r_const])
    K.op("dve", lambda e: e.tensor_copy(out=S1, in_=modfm[:, 0:8]), reads=[r_mod], writes=[r_const])

    def ada_late_1():
        ada_rows([4, 5], g1b, r_g1)

    def ada_late_2():
        ada_fm([6, 7, 8, 9], 24, 40)
        K.op("dve", lambda e: e.scalar_tensor_tensor(out=A2, in0=modfm[:, 32:40], scalar=1.0, in1=N2, op0=ALU.add, op1=ALU.mult),
             reads=[r_const, r_mod], writes=[r_a2])
        K.op("dve", lambda e: e.tensor_copy(out=S2, in_=modfm[:, 24:32]), reads=[r_mod], writes=[r_a2])
        ada_rows([10, 11], g2b, r_g2)

    dump("modfm", modfm[:], [r_mod], F32)
    dump("R2", R2[:].rearrange("p g t -> p (g t)"), [r_const], F32)
    dump("cst", cst[:], [r_const], F32)

    def stop_if(name):
        if upto == name:
            raise _Stop()

    def bc8(v):
        return v.unsqueeze(2).to_broadcast([128, 8, 128])

    def rstd_pool(src_ap, src_regs, col, junk_ap, junk_r):
        ss = small[:, col:col + 1]
        rr = r_sm[col]
        K.op("act", lambda e: e.activation(out=junk_ap, in_=src_ap, func=AF.Square, accum_out=ss),
             reads=list(src_regs), writes=[junk_r, rr])
        K.op("dve", lambda e: e.tensor_scalar(out=ss, in0=ss, scalar1=1.0 / D, scalar2=EPS, op0=ALU.mult, op1=ALU.add),
             reads=[rr], writes=[rr])
        K.op("pool", lambda e: e.tensor_tensor(out=ss, in0=ss, in1=small[:, 120:121], op=ALU.pow),
             reads=[rr, r_const], writes=[rr])
        return ss, rr

    def rstd_of(src_ap, src_regs, col, junk_ap, junk_r):
        ss = small[:, col:col + 1]
        rr = r_sm[col]
        K.op("act", lambda e: e.activation(out=junk_ap, in_=src_ap, func=AF.Square, accum_out=ss),
             reads=list(src_regs), writes=[junk_r, rr])
        K.op("act", lambda e: e.activation(out=ss, in_=ss, func=AF.Ln, scale=1.0 / D, bias=EPS),
             reads=[rr], writes=[rr])
        K.op("act", lambda e: e.activation(out=ss, in_=ss, func=AF.Exp, scale=-0.5),
             reads=[rr], writes=[rr])
        return ss, rr

    def stage_A_act(st, blk):
        t0 = st * STT
        xin, xin_r = f32a.next()
        K.dma("sp", lambda e: e.dma_start(out=xin[:], in_=x[t0 + blk * 128: t0 + (blk + 1) * 128, :]), reads=[], writes=[xin_r])
        xs, xs_r = xsb.next()
        rs, rs_r = rstd_pool(xin[:], [xin_r], blk % 8, xs[:], xs_r)
        K.op("act", lambda e: e.activation(out=xs[:], in_=xin[:], func=AF.Copy, scale=rs), reads=[xin_r, rs_r], writes=[xs_r])
        return xs, xs_r

    def stage_A_pe(st, blk, xs, xs_r):
        pb, pbr = nb(1)
        for kc in range(8):
            K.op("pe", lambda e, kc=kc: e.transpose(out=bank_bf(pb)[:, kc * 128:(kc + 1) * 128],
                                                    in_=xs[:, kc * 128:(kc + 1) * 128], identity=ident_b[:]),
                 reads=[xs_r, r_const], writes=pbr, inc=(kc == 7))
        for kc in range(8):
            K.op("dve", lambda e, kc=kc: e.tensor_scalar(
                out=hT[:, kc, blk * 128:(blk + 1) * 128], in0=bank_bf(pb)[:, kc * 128:(kc + 1) * 128],
                scalar1=A1[:, kc:kc + 1], scalar2=S1[:, kc:kc + 1], op0=ALU.mult, op1=ALU.add),
                 reads=pbr + [r_const], writes=[r_hT[blk]])

    def stage_A(st):
        prev = None
        for blk in range(NBLK):
            cur = stage_A_act(st, blk)
            if prev is not None:
                stage_A_pe(st, blk - 1, *prev)
            prev = cur
        stage_A_pe(st, NBLK - 1, *prev)

    def body(st):
        t0 = st * STT
        stop_if("setup")
        if st == 0 or NOHOIST:
            stage_A(st)
        stop_if("A")
        if st == 0:
            dump("hT", hT[:].rearrange("p k t -> p (k t)"), r_hT, BF16)
        for j in range(2):
            slot, sr = load_unit(wview(w_in, 1536 + 512 * j, 512))
            w8 = s8(slot)
            for c in range(2):
                for fl in range(4):
                    pb, pbr = nb(1)
                    for kc in range(8):
                        K.op("pe", lambda e, kc=kc, w8=w8, fl=fl, c=c, pb=pb: e.matmul(
                            bank(pb), w8[:, kc, fl * 128:(fl + 1) * 128], hT[:, kc, c * 512:(c + 1) * 512],
                            start=(kc == 0), stop=(kc == 7)),
                             reads=[sr] + r_hT[4 * c:4 * c + 4], writes=pbr, inc=(kc == 7))
                    K.op("act", lambda e, pb=pb, j=j, fl=fl, c=c: e.activation(
                        out=B1[:, 4 * j + fl, c * 512:(c + 1) * 512], in_=bank(pb), func=AF.Gelu),
                         reads=pbr, writes=W_B1)
        vs = [load_unit(wview(w_in, 2560 + 512 * j, 512)) for j in range(2)]
        qs_pre = [load_unit(wview(w_in, 512 * j, 512)) for j in range(2)]
        i_k = scur[0] % NSLOT
        scur[0] += 1
        kslot, ksr = slots[i_k], r_slot[i_k]
        for dup in range(2):
            for kv in range(4):
                c0 = kv * 128 + dup * 64
                dstv = s8(kslot)[:, :, c0:c0 + 64]
                srcv = wview(w_in, 1024 + kv * 64, 64)
                K.dma("pool", lambda e, d=dstv, s=srcv: e.dma_start(out=d, in_=s), reads=[], writes=[ksr], parallel=(dup + kv > 0))
        v_pre = load_unit(wview(w_in, 1280, 256), width=2048, view=lambda s_: s_[:, 0:2048].rearrange("p (k f) -> p k f", k=8))
        def vg_block(blk):
            gv, gv_r = f32a.next()
            for j in range(2):
                pb, pbr = nb(1)
                w8 = s8(vs[j][0])
                for kc in range(8):
                    K.op("pe", lambda e, kc=kc: e.matmul(
                        bank(pb), hT[:, kc, blk * 128:(blk + 1) * 128], w8[:, kc, :], start=(kc == 0), stop=(kc == 7)),
                         reads=[vs[j][1], r_hT[blk]], writes=pbr, inc=(kc == 7))
                K.op("act", lambda e: e.activation(out=gv[:, 512 * j:512 * (j + 1)], in_=bank(pb), func=AF.Gelu),
                     reads=pbr, writes=[gv_r])
            so_ = 16 + 16 * (blk % 4)
            lr = r_sm[16 + blk % 4]
            for j in range(2):
                K.op("dve", lambda e, j=j: e.bn_stats(out=small[:, so_ + 6 * j:so_ + 6 + 6 * j], in_=gv[:, 512 * j:512 * (j + 1)]),
                     reads=[gv_r], writes=[lr])
            K.op("dve", lambda e: e.bn_aggr(out=small[:, so_ + 12:so_ + 14], in_=small[:, so_:so_ + 12]), reads=[lr], writes=[lr])
            K.op("dve", lambda e: e.tensor_scalar(out=small[:, so_ + 14:so_ + 15], in0=small[:, so_ + 13:so_ + 14],
                                                  scalar1=EPS, scalar2=None, op0=ALU.add), reads=[lr], writes=[lr])
            K.op("pool", lambda e: e.tensor_tensor(out=small[:, so_ + 15:so_ + 16], in0=small[:, so_ + 14:so_ + 15],
                                                   in1=small[:, 120:121], op=ALU.pow), reads=[lr, r_const], writes=[lr])
            K.op("dve", lambda e: e.tensor_scalar(
                out=B2[:, blk, :], in0=gv[:], scalar1=small[:, so_ + 12:so_ + 13], scalar2=small[:, so_ + 15:so_ + 16],
                op0=ALU.subtract, op1=ALU.mult), reads=[gv_r, lr], writes=W_B2)
            return gv, gv_r

        def sp_block(blk, tmp, tmp_r):
            b0, b0r = nb(2)
            for g in range(8):
                K.op("pe", lambda e, g=g: e.matmul(
                    PS[:, b0 * 512 + g * 128: b0 * 512 + (g + 1) * 128], B2[:, blk, g * 128:(g + 1) * 128], WmT[:, g, :],
                    start=True, stop=True), reads=[r_B2, r_const], writes=[b0r[g // 4]], inc=(g % 4 == 3))
            t3 = tmp[:].rearrange("p (g t) -> p g t", g=8)
            K.op("dve", lambda e: e.tensor_tensor(
                out=t3, in0=PS[:, b0 * 512:(b0 + 2) * 512].rearrange("p (g t) -> p g t", g=8), in1=bc8(GG), op=ALU.mult),
                 reads=b0r + [r_const], writes=[tmp_r])
            K.op("dve", lambda e: e.tensor_tensor(out=t3, in0=t3, in1=R2[:], op=ALU.add), reads=[tmp_r, r_const], writes=[tmp_r])
            K.op("dve", lambda e: e.tensor_tensor(
                out=B1[:, :, blk * 128:(blk + 1) * 128], in0=t3, in1=B1[:, :, blk * 128:(blk + 1) * 128], op=ALU.mult),
                 reads=[tmp_r, r_B1], writes=W_B1)

        LOOK = 2
        gvs = {}
        for blk in range(NBLK + LOOK):
            if blk < NBLK:
                gvs[blk] = vg_block(blk)
            if blk - LOOK >= 0:
                sp_block(blk - LOOK, *gvs.pop(blk - LOOK))
        stop_if("B1")

        if st == 0:
            dump("ygT_", BB[:, 0:8192], [r_B1], BF16) if False else None
        stop_if("B")
        if st == 0:
            dump("ygT", BB[:, 0:8192], [r_B1], BF16)
        def qk_norm(pb, pbr, gain, dst_ap, dst_regs):
            sq, sq_r = bfh.next()
            K.op("act", lambda e: e.activation(out=sq[:], in_=bank(pb), func=AF.Square), reads=pbr, writes=[sq_r])
            p2, p2r = nb(1)
            K.op("pe", lambda e: e.matmul(bank(p2), bones[:], sq[:], start=True, stop=True),
                 reads=[sq_r, r_const], writes=p2r)
            rq, rq_r = f32h.next()
            K.op("act", lambda e: e.activation(out=rq[:], in_=bank(p2), func=AF.Ln, bias=64.0 * EPS), reads=p2r, writes=[rq_r])
            K.op("act", lambda e: e.activation(out=rq[:], in_=rq[:], func=AF.Exp, scale=-0.5), reads=[rq_r], writes=[rq_r])
            K.op("dve", lambda e: e.scalar_tensor_tensor(out=dst_ap, in0=bank(pb), scalar=gain, in1=rq[:],
                                                         op0=ALU.mult, op1=ALU.mult),
                 reads=pbr + [rq_r, r_const], writes=dst_regs)

        qk_pending = []

        def qk_task(w8, col, c, sr, gain, dst, dst_regs):
            pb, pbr = nb(1)
            for kc in range(8):
                K.op("pe", lambda e, kc=kc: e.matmul(
                    bank(pb), w8[:, kc, col:col + 128], hT[:, kc, c * 512:(c + 1) * 512],
                    start=(kc == 0), stop=(kc == 7)),
                     reads=[sr] + r_hT[4 * c:4 * c + 4], writes=pbr, inc=(kc == 7))
            if qk_pending:
                qk_norm(*qk_pending.pop())
            qk_pending.append((pb, pbr, gain, dst, dst_regs))

        for j in range(2):
            slot, sr = qs_pre[j]
            w8 = s8(slot)
            for c in range(2):
                for fl in range(4):
                    qk_task(w8, fl * 128, c, sr, QG, B3[:, 4 * j + fl, c * 512:(c + 1) * 512], [r_B3])
        w8 = s8(kslot)
        for c in range(2):
            for kv in range(4):
                qk_task(w8, kv * 128, c, ksr, KG, kT[:, kv, 128 + c * 512:128 + (c + 1) * 512], [r_kT])
        qk_norm(*qk_pending.pop())
        slot, sr = v_pre
        wv8 = slot[:, 0:2048].rearrange("p (k f) -> p k f", k=8)
        for blk in range(NBLK):
            pb, pbr = nb(1)
            for kc in range(8):
                K.op("pe", lambda e, kc=kc, blk=blk, pb=pb: e.matmul(
                    bank(pb, 256), hT[:, kc, blk * 128:(blk + 1) * 128], wv8[:, kc, :], start=(kc == 0), stop=(kc == 7)),
                     reads=[sr, r_hT[blk]], writes=pbr, inc=(kc == 7))
            K.op("act", lambda e, blk=blk, pb=pb: e.activation(
                out=vaug[:, 1 + blk, :, 0:64], in_=bank(pb, 256).rearrange("p (v d) -> p v d", v=4), func=AF.Copy),
                 reads=pbr, writes=[r_v])
        stop_if("C1")
        if st == 0:
            dump("qT", B3[:].rearrange("p k t -> p (k t)"), [r_B3], BF16)
            dump("kT", kT[:].rearrange("p k t -> p (k t)"), [r_kT], BF16)
            dump("vaug", vaug[:].rearrange("p b v d -> p (b v d)"), [r_v], BF16)
        def att_qk(blk, g, js):
            pts = {}
            lo = 256 if len(js) == 1 else 0
            for i in range(2):
                pb, pbr = nbhi()
                for jj in js:
                    koff = blk * 128 + jj * 128
                    K.op("pe", lambda e, jj=jj, koff=koff: e.matmul(
                        bank(pb, 256, 256 * jj).rearrange("p (c q) -> p c q", c=2),
                        kT[64 * i:64 * i + 64, g, koff:koff + 128],
                        B3[64 * i:64 * i + 64, 2 * g:2 * g + 2, blk * 128:(blk + 1) * 128],
                        start=True, stop=True), reads=[r_kT, r_B3], writes=pbr, inc=(jj == 1))
                pe_, pe_r = bfh.next()
                K.op("act", lambda e: e.activation(out=pe_[:, lo:512], in_=bank(pb, 512 - lo, lo), func=AF.Exp, scale=8.0),
                     reads=pbr, writes=[pe_r])
                pt, pt_r = ptp.next()
                eo = (g * 2 + i) * 512
                K.op("dve", lambda e: e.tensor_tensor(out=pt[:, lo:512], in0=pe_[:, lo:512], in1=etab[:, eo + lo:eo + 512], op=ALU.mult),
                     reads=[pe_r, r_const], writes=[pt_r])
                pts[i] = (pt, pt_r)
            return pts

        def att_pv(blk, g, js, pts, yps, ybr):
            for hl in range(4):
                h = 4 * g + hl
                i = h % 2
                cc = (h % 4) // 2
                pt, pt_r = pts[i]
                for n_, jj in enumerate(js):
                    col = jj * 256 + cc * 128
                    K.op("pe", lambda e, n_=n_, jj=jj, col=col: e.matmul(
                        yps[:, h % 8, 0:65], pt[:, col:col + 128], vaug[:, blk + jj, g, 0:65],
                        start=(n_ == 0), stop=(n_ == len(js) - 1)),
                         reads=[pt_r, r_v], writes=[ybr[(h % 8) // 4]], inc=(n_ == len(js) - 1 and hl == 3))

        def att_half(blk, half, yps, ybr, yt, yt_r):
            den = small[:, 80 + 8 * ((2 * blk + half) % 4):88 + 8 * ((2 * blk + half) % 4)]
            dr = r_sm[24 + (2 * blk + half) % 4]
            K.op("dve", lambda e: e.tensor_tensor(out=den.unsqueeze(2), in0=yps[:, :, 64:65],
                                                  in1=esink[:, 8 * half:8 * half + 8].unsqueeze(2), op=ALU.add),
                 reads=ybr + [r_const], writes=[dr])
            K.op("dve", lambda e: e.reciprocal(out=den, in_=den), reads=[dr], writes=[dr])
            K.op("dve", lambda e: e.tensor_tensor(
                out=yt[:, 512 * half:512 * (half + 1)].rearrange("p (h d) -> p h d", h=8), in0=yps[:, :, 0:64],
                in1=den.unsqueeze(2).to_broadcast([128, 8, 64]), op=ALU.mult),
                 reads=ybr + [dr], writes=[yt_r])

        def att_fin(blk, yt, yt_r):
            pb, pbr = nbhi()
            for kc in range(8):
                K.op("pe", lambda e, kc=kc: e.transpose(out=bank_bf(pb)[:, kc * 128:(kc + 1) * 128],
                                                        in_=yt[:, kc * 128:(kc + 1) * 128], identity=ident_b[:]),
                     reads=[yt_r, r_const], writes=pbr, inc=(kc == 7))
            K.op("act", lambda e: e.activation(
                out=B2[:, :, blk * 128:(blk + 1) * 128], in_=bank_bf(pb).rearrange("p (k t) -> p k t", k=8), func=AF.Copy),
                 reads=pbr, writes=W_B2)

        ybr = bank_r[0:2]
        yps = PS[:, 0:1024].rearrange("p (h d) -> p h d", h=8)
        units = [(blk, g) for blk in range(NBLK) for g in range(4)]
        jsof = lambda blk: [1] if (st * NBLK + blk) == 0 else [0, 1]
        ALOOK = 2
        pend = {}
        yts = {}
        for n in range(len(units) + ALOOK):
            if n < len(units):
                b2, g2 = units[n]
                pend[n] = att_qk(b2, g2, jsof(b2))
            m_ = n - ALOOK
            if m_ >= 0:
                blk, g = units[m_]
                if g == 0:
                    yts[blk] = ytm.next()
                att_pv(blk, g, jsof(blk), pend.pop(m_), yps, ybr)
                if g % 2 == 1:
                    att_half(blk, g // 2, yps, ybr, *yts[blk])
                if g == 3:
                    att_fin(blk, *yts.pop(blk))
        stop_if("C")
        if st == 0:
            dump("yaT", BB[:, 8192:16384], [r_B2], BF16)
        K.op("dve", lambda e: e.tensor_copy(out=kT[:, :, 0:128], in_=kT[:, :, 1024:1152]), reads=[r_kT], writes=[r_kT])
        K.op("dve", lambda e: e.tensor_copy(out=vaug[:, 0, :, :], in_=vaug[:, 8, :, :]), reads=[r_v], writes=[r_v])

        if st == 0:
            ada_late_1()
        for j in range(2):
            for br_i, (Wo, src, src_r, gcol) in enumerate(((w_oa, B2, r_B2, 3584), (w_og, B1, r_B1, 4608))):
                so, sor = load_unit(wview(Wo, 512 * j, 512))
                sg_, sgr = load_unit(wview(w_in, gcol + 512 * j, 512))
                wo8 = s8(so); wg8 = s8(sg_)
                for c in range(2):
                    for fl in range(4):
                        fch = 4 * j + fl
                        pa, par = nb(1)
                        for kc in range(8):
                            K.op("pe", lambda e, kc=kc, wo8=wo8, fl=fl, c=c, pa=pa, src=src: e.matmul(
                                bank(pa), wo8[:, kc, fl * 128:(fl + 1) * 128], src[:, kc, c * 512:(c + 1) * 512],
                                start=(kc == 0), stop=(kc == 7)), reads=[sor, src_r], writes=par, inc=(kc == 7))
                        pg, pgr = nb(1)
                        for kc in range(8):
                            K.op("pe", lambda e, kc=kc, wg8=wg8, fl=fl, c=c, pg=pg: e.matmul(
                                bank(pg), wg8[:, kc, fl * 128:(fl + 1) * 128], hT[:, kc, c * 512:(c + 1) * 512],
                                start=(kc == 0), stop=(kc == 7)), reads=[sgr] + r_hT[4 * c:4 * c + 4], writes=pgr, inc=(kc == 7))
                        sg, sg_r = f32h.next()
                        bcol = BG[:, 8 * br_i + fch: 8 * br_i + fch + 1]
                        K.op("act", lambda e, sg=sg, pg=pg, bcol=bcol: e.activation(out=sg[:], in_=bank(pg), func=AF.Sigmoid, bias=bcol),
                             reads=pgr + [r_const], writes=[sg_r])
                        dst = B3[:, fch, c * 512:(c + 1) * 512]
                        if br_i == 0:
                            K.op("dve", lambda e, sg=sg, pa=pa, dst=dst: e.tensor_tensor(out=dst, in0=bank(pa), in1=sg[:], op=ALU.mult),
                                 reads=par + [sg_r], writes=[r_B3])
                        else:
                            t2, t2_r = f32h.next()
                            K.op("dve", lambda e, sg=sg, pa=pa, t2=t2: e.tensor_tensor(out=t2[:], in0=bank(pa), in1=sg[:], op=ALU.mult),
                                 reads=par + [sg_r], writes=[t2_r])
                            K.op("dve", lambda e, t2=t2, dst=dst: e.tensor_tensor(out=dst, in0=t2[:], in1=dst, op=ALU.add),
                                 reads=[t2_r, r_B3], writes=[r_B3])

        stop_if("D")
        if st == 0:
            dump("mergedT", B3[:].rearrange("p k t -> p (k t)"), [r_B3], BF16)
        if st == 0:
            ada_late_2()
        for j in range(2):
            slot, sr = load_unit(wview(w_out, 512 * j, 512))
            w8 = s8(slot)
            K.op("dve", lambda e, w8=w8, j=j: e.tensor_tensor(
                out=w8, in0=w8, in1=g1b[:, 512 * j:512 * (j + 1)].unsqueeze(1).to_broadcast([128, 8, 512]), op=ALU.mult),
                 reads=[sr, r_g1], writes=[sr])
            for blk in range(NBLK):
                xh, xh_r = f32h.next()
                K.dma("sp", lambda e, xh=xh, blk=blk, j=j: e.dma_start(
                    out=xh[:], in_=x[t0 + blk * 128: t0 + (blk + 1) * 128, 512 * j:512 * (j + 1)]), reads=[], writes=[xh_r])
                pb, pbr = nb(1)
                for kc in range(8):
                    K.op("pe", lambda e, kc=kc, w8=w8, blk=blk, pb=pb: e.matmul(
                        bank(pb), B3[:, kc, blk * 128:(blk + 1) * 128], w8[:, kc, :], start=(kc == 0), stop=(kc == 7)),
                         reads=[sr, r_B3], writes=pbr, inc=(kc == 7))
                K.op("dve", lambda e, xh=xh, pb=pb, blk=blk, j=j: e.tensor_tensor(
                    out=acc[:, blk, 512 * j:512 * (j + 1)], in0=bank(pb), in1=xh[:], op=ALU.add),
                     reads=pbr + [xh_r], writes=[r_acc[blk], r_B1 if blk < 4 else r_B2])

        stop_if("E")
        if st == 0:
            dump("xn", BB[:].bitcast(F32), r_acc, F32)
        rt, rt_r = rt_tile, r_rt
        GL = rt[:, 0:32].rearrange("p (b g) -> p b g", b=8)
        EL = rt[:, 32:160]
        def f_head(blk):
            xs32, xs32_r = f32a.next()
            rs, rs_r = rstd_of(acc[:, blk, :], [r_acc[blk]], 8 + blk % 8, xs32[:], xs32_r)
            K.op("act", lambda e: e.activation(out=xs32[:], in_=acc[:, blk, :], func=AF.Copy, scale=rs),
                 reads=[r_acc[blk], rs_r], writes=[xs32_r])
            b0, b0r = nb(2)
            for kc in range(8):
                K.op("pe", lambda e, kc=kc: e.transpose(
                    out=PS[:, b0 * 512 + kc * 128: b0 * 512 + (kc + 1) * 128], in_=xs32[:, kc * 128:(kc + 1) * 128],
                    identity=ident_f[:]), reads=[xs32_r, r_const], writes=[b0r[kc // 4]], inc=(kc % 4 == 3))
            h2f, h2f_r = xs32, xs32_r
            h3 = h2f[:].rearrange("p (k t) -> p k t", k=8)
            K.op("dve", lambda e: e.tensor_tensor(
                out=h3, in0=PS[:, b0 * 512:(b0 + 2) * 512].rearrange("p (k t) -> p k t", k=8), in1=bc8(A2), op=ALU.mult),
                 reads=b0r + [r_a2], writes=[h2f_r])
            K.op("dve", lambda e: e.tensor_tensor(out=h3, in0=h3, in1=bc8(S2), op=ALU.add),
                 reads=[h2f_r, r_a2], writes=[h2f_r])
            return h3, h2f_r

        def f_tail(blk, h3, h2f_r):
            K.op("act", lambda e: e.activation(out=B3[:, :, blk * 128:(blk + 1) * 128], in_=h3, func=AF.Copy),
                 reads=[h2f_r], writes=[r_B3])
            pb, pbr = nb(1)
            for kc in range(8):
                K.op("pe", lambda e, kc=kc: e.matmul(bank(pb, 20), h3[:, kc, :], wr_sb[:, kc, :], start=(kc == 0), stop=(kc == 7)),
                     reads=[h2f_r, r_const], writes=pbr, inc=(kc == 7))
            K.op("dve", lambda e: e.tensor_tensor(out=GL[:, blk, :], in0=bank(pb, 4), in1=br_b[:, 0:4], op=ALU.add),
                 reads=pbr + [r_const], writes=[rt_r])
            K.op("dve", lambda e: e.tensor_tensor(out=EL[:, 16 * blk:16 * blk + 16], in0=bank(pb, 16, 4), in1=br_b[:, 4:20], op=ALU.add),
                 reads=pbr + [r_const], writes=[rt_r])

        prev = None
        for blk in range(NBLK):
            cur = f_head(blk)
            if prev is not None:
                f_tail(blk - 1, *prev)
            prev = cur
        f_tail(NBLK - 1, *prev)
        o = 160

        def v8(n):
            nonlocal o
            a = rt[:, o:o + 8 * n].rearrange("p (b n) -> p b n", b=8)
            o += 8 * n
            return a

        ngm = v8(1); T4 = v8(4); goh = v8(4); gex = v8(4); gsum = v8(1); gw = v8(1)
        TM = v8(16); esel = v8(4); nm1 = v8(1); T5 = v8(4); oh1 = v8(4); e2 = v8(4); nm2 = v8(1); oh2 = v8(4)
        dd = v8(1); ed = v8(1); w1 = v8(1); w2 = v8(1); ta = v8(4); wig = v8(4); comb = v8(16)
        RS = [rt_r]

        def sop(fn, eng="dve"):
            K.op(eng, fn, reads=RS, writes=RS)

        def b4(a):
            return a.to_broadcast([128, 8, 4])

        sop(lambda e: e.tensor_reduce(out=ngm, in_=GL, axis=AX.X, op=ALU.max, negate=True))
        sop(lambda e: e.tensor_tensor(out=T4, in0=GL, in1=b4(ngm), op=ALU.add))
        sop(lambda e: e.tensor_single_scalar(out=goh, in_=T4, scalar=0.0, op=ALU.is_ge))
        sop(lambda e: e.activation(out=gex, in_=T4, func=AF.Exp), eng="act")
        sop(lambda e: e.tensor_reduce(out=gsum, in_=gex, axis=AX.X, op=ALU.add))
        sop(lambda e: e.reciprocal(out=gw, in_=gsum))
        for g in range(4):
            sop(lambda e, g=g: e.tensor_tensor(out=TM[:, :, 4 * g:4 * g + 4],
                                               in0=EL.rearrange("p (b n) -> p b n", b=8)[:, :, 4 * g:4 * g + 4],
                                               in1=b4(goh[:, :, g:g + 1]), op=ALU.mult))
        sop(lambda e: e.tensor_tensor(out=esel, in0=TM[:, :, 0:4], in1=TM[:, :, 4:8], op=ALU.add))
        sop(lambda e: e.tensor_tensor(out=esel, in0=esel, in1=TM[:, :, 8:12], op=ALU.add))
        sop(lambda e: e.tensor_tensor(out=esel, in0=esel, in1=TM[:, :, 12:16], op=ALU.add))
        sop(lambda e: e.tensor_reduce(out=nm1, in_=esel, axis=AX.X, op=ALU.max, negate=True))
        sop(lambda e: e.tensor_tensor(out=T5, in0=esel, in1=b4(nm1), op=ALU.add))
        sop(lambda e: e.tensor_single_scalar(out=oh1, in_=T5, scalar=0.0, op=ALU.is_ge))
        sop(lambda e: e.scalar_tensor_tensor(out=e2, in0=oh1, scalar=-1e30, in1=esel, op0=ALU.mult, op1=ALU.add))
        sop(lambda e: e.tensor_reduce(out=nm2, in_=e2, axis=AX.X, op=ALU.max, negate=True))
        sop(lambda e: e.tensor_tensor(out=T5, in0=e2, in1=b4(nm2), op=ALU.add))
        sop(lambda e: e.tensor_single_scalar(out=oh2, in_=T5, scalar=0.0, op=ALU.is_ge))
        sop(lambda e: e.tensor_tensor(out=dd, in0=nm1, in1=nm2, op=ALU.subtract))
        sop(lambda e: e.activation(out=ed, in_=dd, func=AF.Exp), eng="act")
        sop(lambda e: e.tensor_single_scalar(out=w1, in_=ed, scalar=1.0, op=ALU.add))
        sop(lambda e: e.reciprocal(out=w1, in_=w1))
        sop(lambda e: e.tensor_tensor(out=w1, in0=w1, in1=gw, op=ALU.mult))
        sop(lambda e: e.tensor_tensor(out=w2, in0=w1, in1=ed, op=ALU.mult))
        sop(lambda e: e.tensor_tensor(out=ta, in0=oh1, in1=b4(w1), op=ALU.mult))
        sop(lambda e: e.tensor_tensor(out=wig, in0=oh2, in1=b4(w2), op=ALU.mult))
        sop(lambda e: e.tensor_tensor(out=wig, in0=wig, in1=ta, op=ALU.add))
        for g in range(4):
            sop(lambda e, g=g: e.tensor_tensor(out=comb[:, :, 4 * g:4 * g + 4], in0=wig, in1=b4(goh[:, :, g:g + 1]), op=ALU.mult))
        for blk in range(NBLK):
            pc, pcr = nb(1)
            K.op("pe", lambda e, pc=pc, blk=blk: e.transpose(out=PS[0:16, pc * 512: pc * 512 + 128], in_=comb[:, blk, :],
                                                            identity=ident_f[:]),
                 reads=RS + [r_const], writes=pcr)
            K.op("act", lambda e, pc=pc, blk=blk: e.activation(out=combT[0:16, blk * 128:(blk + 1) * 128],
                                                              in_=PS[0:16, pc * 512: pc * 512 + 128], func=AF.Copy),
                 reads=pcr, writes=[r_comb])

        stop_if("F")
        if st == 0:
            dump("h2T", B3[:].rearrange("p k t -> p (k t)"), [r_B3], BF16)
            dump("combT", combT[:], [r_comb], BF16)
        def moe_gu(ex, c, wg8, wu8, sgr, sur):
            hid, hid_r = hidp.next()
            cb, cb_r = bfh.next()
            for fl in range(4):
                pg, pgr = nb(1)
                for kc in range(8):
                    K.op("pe", lambda e, kc=kc: e.matmul(
                        bank(pg), wg8[:, kc, fl * 128:(fl + 1) * 128], B3[:, kc, c * 512:(c + 1) * 512],
                        start=(kc == 0), stop=(kc == 7)), reads=[sgr, r_B3], writes=pgr, inc=(kc == 7))
                pu, pur = nb(1)
                for kc in range(8):
                    K.op("pe", lambda e, kc=kc: e.matmul(
                        bank(pu), wu8[:, kc, fl * 128:(fl + 1) * 128], B3[:, kc, c * 512:(c + 1) * 512],
                        start=(kc == 0), stop=(kc == 7)), reads=[sur, r_B3], writes=pur, inc=(kc == 7))
                if fl == 0:
                    pcb, pcbr = nb(1)
                    K.op("pe", lambda e: e.matmul(bank(pcb), selb[0:16, ex, :], combT[0:16, c * 512:(c + 1) * 512],
                                                  start=True, stop=True), reads=[r_comb, r_const], writes=pcbr)
                    K.op("act", lambda e: e.activation(out=cb[:], in_=bank(pcb), func=AF.Copy), reads=pcbr, writes=[cb_r])
                sl, sl_r = f32h.next()
                K.op("act", lambda e: e.activation(out=sl[:], in_=bank(pg), func=AF.Silu), reads=pgr, writes=[sl_r])
                K.op("dve", lambda e: e.tensor_tensor(out=sl[:], in0=bank(pu), in1=sl[:], op=ALU.mult),
                     reads=pur + [sl_r], writes=[sl_r])
                K.op("dve", lambda e: e.tensor_tensor(out=hid[:, fl, :], in0=sl[:], in1=cb[:], op=ALU.mult),
                     reads=[sl_r, cb_r], writes=[hid_r])
            return hid, hid_r

        def moe_dn(ex, c, hid, hid_r, wd4, sdr):
            for tt in range(4):
                blk = 4 * c + tt
                for dh in range(2):
                    pd, pdr = nb(1)
                    for fl in range(4):
                        K.op("pe", lambda e, fl=fl: e.matmul(
                            bank(pd), hid[:, fl, tt * 128:(tt + 1) * 128], wd4[:, fl, dh * 512:(dh + 1) * 512],
                            start=(fl == 0), stop=(fl == 3)), reads=[hid_r, sdr], writes=pdr, inc=(fl == 3))
                    K.op("dve", lambda e: e.tensor_tensor(
                        out=acc[:, blk, 512 * dh:512 * (dh + 1)], in0=bank(pd), in1=acc[:, blk, 512 * dh:512 * (dh + 1)], op=ALU.add),
                         reads=pdr + [r_acc[blk]], writes=[r_acc[blk]])
                if ex == 15:
                    K.dma("sp", lambda e: e.dma_start(out=out[t0 + blk * 128: t0 + (blk + 1) * 128, :], in_=acc[:, blk, :]),
                          reads=[r_acc[blk]], writes=[])

        for ex in range(16):
            sg_, sgr = load_unit(w_eg[ex].rearrange("(kc p) f -> p kc f", p=128))
            su_, sur = load_unit(w_eu[ex].rearrange("(kc p) f -> p kc f", p=128))
            sd_, sdr = load_unit(w_ed[ex].rearrange("(fc p) d -> p fc d", p=128),
                                 view=lambda s_: s_[:].rearrange("p (k f) -> p k f", k=4))
            wg8 = s8(sg_); wu8 = s8(su_)
            wd4 = sd_[:].rearrange("p (k f) -> p k f", k=4)
            hoist = (ex >= 8 and st + 1 < n_st and upto is None and not NOHOIST)
            if hoist:
                a_nxt = stage_A_act(st + 1, ex - 8)
            h0 = moe_gu(ex, 0, wg8, wu8, sgr, sur)
            K.op("dve", lambda e: e.tensor_tensor(out=wd4, in0=wd4, in1=g2b[:].unsqueeze(1).to_broadcast([128, 4, 1024]),
                                                  op=ALU.mult), reads=[sdr, r_g2], writes=[sdr])
            h1 = moe_gu(ex, 1, wg8, wu8, sgr, sur)
            moe_dn(ex, 0, h0[0], h0[1], wd4, sdr)
            if hoist:
                stage_A_pe(st + 1, ex - 8, *a_nxt)
            moe_dn(ex, 1, h1[0], h1[1], wd4, sdr)

    try:
        for st_ in range(n_st):
            body(st_)
    except _Stop:
        pass
    K.finish("sp")
    sems = {k: es.enter_context(nc.semaphore(k)) for k in K.sem_names()}
    block = es.enter_context(nc.Block())
    K.replay(nc, block, sems)
    es.close()
    return nc


def host_inputs(inp, b):
    f = lambda a: np.ascontiguousarray(a, dtype=np.float32)
    fm = lambda v: f(np.asarray(v).reshape(-1, 128).T)
    m = {}
    m["x"] = f(inp["x"][b])
    m["c_fm"] = fm(inp["c"][b])
    m["w_ada"] = f(inp["w_ada"][0])
    m["b_ada_fm"] = fm(inp["b_ada"][0])
    m["b_ada_row"] = f(inp["b_ada"][0][None, :])
    m["n1_fm"] = fm(inp["norm1_gain"][0])
    m["n2_fm"] = fm(inp["norm2_gain"][0])
    m["w_in"] = f(inp["w_in"][0])
    m["bgate_fm"] = fm(inp["b_branch_gate"][0])
    m["qg_fm"] = f(np.tile(inp["q_norm_gain"][0], 2)[:, None])
    m["kg_fm"] = f(np.tile(inp["k_norm_gain"][0], 2)[:, None])
    m["sinks_row"] = f(inp["attn_sinks"][0][None, :])
    m["gg_fm"] = fm(inp["gmlp_norm_gain"][0])
    m["gb_rows"] = f(inp["gmlp_norm_bias"][0].reshape(8, 128))
    m["wsT"] = f(np.transpose(inp["gmlp_w_spatial"][0], (2, 0, 1)).reshape(128, 1024))
    m["bsp_row"] = f(inp["gmlp_b_spatial"][0].reshape(1, 1024))
    m["w_oa"] = f(inp["w_o_attn"][0])
    m["w_og"] = f(inp["w_o_gmlp"][0])
    m["w_out"] = f(inp["w_out"][0])
    m["wr"] = f(np.concatenate([inp["w_group_router"][0], inp["w_expert_router"][0]], axis=1))
    m["br_row"] = f(np.concatenate([inp["b_group_router"][0], inp["b_expert_router"][0]])[None, :])
    m["w_eg"] = f(inp["w_expert_gate"][0].reshape(16, 1024, 512))
    m["w_eu"] = f(inp["w_expert_up"][0].reshape(16, 1024, 512))
    m["w_ed"] = f(inp["w_expert_down"][0].reshape(16, 512, 1024))
    return m


def host_consts():
    m = {}
    m["k_ident"] = np.eye(128, dtype=np.float32)
    m["k_etab"] = alibi_tables()
    s_idx = np.arange(128)[:, None]
    t_idx = np.arange(128)[None, :]
    m["k_maskT"] = (s_idx <= t_idx).astype(np.float32)
    bo = np.zeros((128, 128), np.float32)
    bo[:64, :64] = 1.0
    bo[64:, 64:] = 1.0
    m["k_bones"] = bo
    sel = np.zeros((16, 16, 128), np.float32)
    for e_ in range(16):
        sel[e_, e_, :] = 1.0
    m["k_sel"] = sel.reshape(16, 2048)
    m["k_ones"] = np.ones((1, 128), np.float32)
    return m


_NC_CACHE = {}


def kernel(**inputs):
    inp = {k: np.asarray(v) for k, v in inputs.items()}
    if "nc" not in _NC_CACHE:
        _NC_CACHE["nc"] = build_nc()
    nc = _NC_CACHE["nc"]
    consts = host_consts()
    in_maps = []
    for b in range(8):
        m = host_inputs(inp, b)
        m.update(consts)
        in_maps.append(m)
    res = run_bass_kernel_spmd(nc, in_maps, core_ids=list(range(8)))
    return np.stack([np.asarray(r["out"]).reshape(S, D) for r in res.results], axis=0).astype(np.float32)
```

```python
import numpy as np
from contextlib import ExitStack
import concourse.bass as bass
import concourse.mybir as mybir
from concourse.bass_utils import run_bass_kernel_spmd

F32 = mybir.dt.float32
BF16 = mybir.dt.bfloat16
AF = mybir.ActivationFunctionType
ALU = mybir.AluOpType
AX = mybir.AxisListType

S = 4096
D = 1024
STT = 1024
NBLK = 8
NSLOT = 6
EPS = 1e-6
NOHOIST = False
HOIST_EX = 11
ENGS = ("pe", "act", "dve", "pool", "sp")


class Region:
    __slots__ = ("name", "last_w", "extra_w", "readers")

    def __init__(self, name):
        self.name = name
        self.last_w = None
        self.extra_w = []
        self.readers = {}


class Sync:
    def __init__(self, n_dma_sp=8, n_dma_pool=6):
        self.prog = {e: [] for e in ENGS}
        self.cnt = {e: 0 for e in ENGS}
        self.waited = {e: {} for e in ENGS}
        self.dma_sems = {"sp": [f"dsp{i}" for i in range(n_dma_sp)],
                         "pool": [f"dpl{i}" for i in range(n_dma_pool)]}
        self.dma_tot = {}
        for q in self.dma_sems:
            for k in self.dma_sems[q]:
                self.dma_tot[k] = 0
        self.dma_rr = {"sp": 0, "pool": 0}
        self.n_ops = 0

    def sem_names(self):
        return list(ENGS) + [k for q in self.dma_sems for k in self.dma_sems[q]]

    def _wait(self, e, ev):
        k, v = ev
        if self.waited[e].get(k, 0) >= v:
            return
        self.waited[e][k] = v
        self.prog[e].append(("wait", k, v))

    def _deps(self, e, reads, writes, parallel=False):
        deps = []
        for r in reads:
            if r.last_w is not None:
                deps.append(r.last_w)
            deps.extend(r.extra_w)
        for w in writes:
            if not parallel:
                if w.last_w is not None:
                    deps.append(w.last_w)
                deps.extend(w.extra_w)
            deps.extend(w.readers.items())
        for ev in deps:
            if ev[0] == e and e == "pe":
                continue
            self._wait(e, ev)

    def _commit(self, ev, reads, writes, parallel=False):
        k, v = ev
        for r in reads:
            if r.readers.get(k, 0) < v:
                r.readers[k] = v
        for w in writes:
            if parallel:
                w.extra_w.append(ev)
            else:
                w.last_w = ev
                w.extra_w = []
            w.readers = {}

    def op(self, e, fn, reads=(), writes=(), inc=True):
        self._deps(e, reads, writes)
        ev = (e, self.cnt[e] + 1)
        if inc:
            self.cnt[e] += 1
            self.prog[e].append(("op", _capture(fn), e, 1))
        else:
            self.prog[e].append(("op", _capture(fn), None, 0))
        self._commit(ev, reads, writes)
        self.n_ops += 1

    def dma(self, q, fn, reads=(), writes=(), parallel=False):
        sems = self.dma_sems[q]
        k = sems[self.dma_rr[q] % len(sems)]
        self.dma_rr[q] += 1
        if self.dma_tot[k] > 0:
            self._wait(q, (k, self.dma_tot[k]))
        self._deps(q, reads, writes, parallel)
        self.dma_tot[k] += 16
        ev = (k, self.dma_tot[k])
        self.prog[q].append(("op", _capture(fn), k, 16))
        self._commit(ev, reads, writes, parallel)
        return ev

    def finish(self, e="sp"):
        for k, v in self.dma_tot.items():
            if v > 0:
                self._wait(e, (k, v))
        for k in ENGS:
            if k != e and self.cnt[k] > 0:
                self._wait(e, (k, self.cnt[k]))

    def replay(self, nc, block, sems):
        handles = {"pe": "tensor", "act": "scalar", "dve": "vector", "pool": "gpsimd", "sp": "sync"}

        def make(e):
            def body(eng):
                for item in self.prog[e]:
                    if item[0] == "wait":
                        eng.wait_ge(sems[item[1]], item[2])
                    else:
                        name, a, k = item[1]
                        ins = getattr(eng, name)(*a, **k)
                        if item[2] is not None:
                            ins.then_inc(sems[item[2]], item[3])
            return body

        for e in ENGS:
            getattr(block, handles[e])(make(e))


class _Rec:
    def __init__(self):
        self.call = None

    def __getattr__(self, name):
        def f(*a, **k):
            self.call = (name, a, k)
            return self
        return f


def _capture(fn):
    r = _Rec()
    fn(r)
    assert r.call is not None
    return r.call


class RR:
    def __init__(self, tiles, name):
        self.tiles = tiles
        self.regs = [Region(f"{name}{i}") for i in range(len(tiles))]
        self.i = 0

    def next(self):
        j = self.i % len(self.tiles)
        self.i += 1
        return self.tiles[j], self.regs[j]


def alibi_tables():
    slopes = np.array([2.0 ** (-8.0 * (i + 1) / 16) for i in range(16)], dtype=np.float64)
    s_idx = np.arange(128)[:, None]
    q_idx = np.arange(128)[None, :]
    E = np.zeros((128, 4, 2, 2, 2, 128), dtype=np.float32)
    for g in range(4):
        for i in range(2):
            for cc in range(2):
                h = 4 * g + 2 * cc + i
                d1 = (q_idx - s_idx).astype(np.float64)
                E[:, g, i, 1, cc, :] = np.where(d1 >= 0, np.exp(-slopes[h] * d1), 0.0)
                d0 = (q_idx + 128 - s_idx).astype(np.float64)
                E[:, g, i, 0, cc, :] = np.where(d0 < 128, np.exp(-slopes[h] * d0), 0.0)
    return E.reshape(128, 4096)


class _Stop(Exception):
    pass


def build_nc(n_st=4, dbg=False, upto=None):
    nc = bass.Bass("TRN2", target_bir_lowering=False)

    def din(name, shape):
        return nc.dram_tensor(name, list(shape), F32, kind="ExternalInput").ap()

    x = din("x", [S, D])
    c_fm = din("c_fm", [128, 8])
    w_ada = din("w_ada", [D, 6 * D])
    b_ada_fm = din("b_ada_fm", [128, 48])
    b_ada_row = din("b_ada_row", [1, 6 * D])
    n1_fm = din("n1_fm", [128, 8])
    n2_fm = din("n2_fm", [128, 8])
    w_in = din("w_in", [D, 5632])
    bgate_fm = din("bgate_fm", [128, 16])
    qg_fm = din("qg_fm", [128, 1])
    kg_fm = din("kg_fm", [128, 1])
    sinks_row = din("sinks_row", [1, 16])
    gg_fm = din("gg_fm", [128, 8])
    gb_rows = din("gb_rows", [8, 128])
    wsT = din("wsT", [128, 1024])
    bsp_row = din("bsp_row", [1, 1024])
    w_oa = din("w_oa", [D, D])
    w_og = din("w_og", [D, D])
    w_out = din("w_out", [D, D])
    wr = din("wr", [D, 20])
    br_row = din("br_row", [1, 20])
    w_eg = din("w_eg", [16, D, 512])
    w_eu = din("w_eu", [16, D, 512])
    w_ed = din("w_ed", [16, 512, D])
    k_ident = din("k_ident", [128, 128])
    k_etab = din("k_etab", [128, 4096])
    k_maskT = din("k_maskT", [128, 128])
    k_bones = din("k_bones", [128, 128])
    k_sel = din("k_sel", [16, 2048])
    k_ones = din("k_ones", [1, 128])
    out = nc.dram_tensor("out", [S, D], F32, kind="ExternalOutput").ap()

    K = Sync()
    es = ExitStack()

    def sb(name, shape, dt):
        return es.enter_context(nc.sbuf_tensor(name, list(shape), dt))

    BB = sb("BB", [128, 16384], BF16)
    B1 = BB[:, 0:8192].rearrange("p (k t) -> p k t", k=8)
    B2 = BB[:, 8192:16384].rearrange("p (k t) -> p k t", k=8)
    acc = BB[:].bitcast(F32).rearrange("p (b d) -> p b d", b=8)
    B3 = sb("B3", [128, 8, 1024], BF16)
    hT = sb("hT", [128, 8, 1024], BF16)
    slots = [sb(f"slot{i}", [128, 4096], BF16) for i in range(NSLOT)]
    kT = sb("kT", [128, 4, 1152], BF16)
    vaug = sb("vaug", [128, 9, 4, 72], BF16)
    etab = sb("etab", [128, 4096], BF16)
    R2 = sb("R2", [128, 8, 128], F32)
    g1b = sb("g1b", [128, 1024], BF16)
    g2b = sb("g2b", [128, 1024], BF16)
    WmT = sb("WmT", [128, 8, 128], BF16)
    ident_f = sb("ident_f", [128, 128], F32)
    ident_b = sb("ident_b", [128, 128], BF16)
    bones = sb("bones", [128, 128], BF16)
    selb = sb("selb", [16, 16, 128], BF16)
    wr_sb = sb("wr_sb", [128, 8, 20], F32)
    br_b = sb("br_b", [128, 20], F32)
    esink = sb("esink", [128, 16], F32)
    combT = sb("combT", [16, 1024], BF16)
    cst = sb("cst", [128, 96], F32)
    modfm = sb("modfm", [128, 48], F32)
    bfm = sb("bfm", [128, 48], F32)
    c_bf = sb("c_bf", [128, 8], BF16)
    c_rep = sb("c_rep", [128, 8, 128], BF16)
    ones_bf = sb("ones_bf", [128, 1], BF16)
    small = sb("small", [128, 128], F32)
    rt_tile = sb("rt_tile", [128, 816], F32)
    r_rt = Region("rt")
    A1 = cst[:, 0:8]; S1 = cst[:, 8:16]; A2 = cst[:, 16:24]; S2 = cst[:, 24:32]
    N1 = cst[:, 32:40]; N2 = cst[:, 40:48]; QG = cst[:, 48:49]; KG = cst[:, 49:50]
    GG = cst[:, 50:58]; BG = cst[:, 58:74]; CF = cst[:, 74:82]; CA = cst[:, 82:90]

    f32a = RR([sb(f"f32a{i}", [128, 1024], F32) for i in range(3)], "f32a")
    f32h = RR([sb(f"f32h{i}", [128, 512], F32) for i in range(5)], "f32h")
    bfh = RR([sb(f"bfh{i}", [128, 512], BF16) for i in range(6)], "bfh")
    ptp = RR([sb(f"pt{i}", [128, 512], BF16) for i in range(6)], "pt")
    xsb = RR([sb(f"xsb{i}", [128, 1024], BF16) for i in range(2)], "xsb")
    ytm = RR([sb(f"ytm{i}", [128, 1024], BF16) for i in range(1)], "ytm")
    hidp = RR([sb(f"hid{i}", [128, 4, 512], BF16) for i in range(2)], "hid")

    PS = es.enter_context(nc.psum_tensor("PS", [128, 4096], F32))
    bank_r = [Region(f"bank{i}") for i in range(8)]
    pcur = [0]

    def nb(n=1):
        c = pcur[0]
        if c % n:
            c += n - (c % n)
        c %= 8
        pcur[0] = c + n
        return c, bank_r[c:c + n]

    hcur = [0]

    def nbhi():
        c = 2 + hcur[0] % 6
        hcur[0] += 1
        return c, bank_r[c:c + 1]

    def bank(i, w=512, off=0):
        return PS[:, i * 512 + off: i * 512 + off + w]

    def bank_bf(i):
        return PS[:, i * 512:(i + 1) * 512].bitcast(BF16)

    r_B1 = Region("B1"); r_B2 = Region("B2"); r_B3 = Region("B3")
    r_hT = [Region(f"hT{i}") for i in range(NBLK)]
    r_acc = [Region(f"acc{i}") for i in range(NBLK)]
    r_slot = [Region(f"slot{i}") for i in range(NSLOT)]
    r_kT = Region("kT"); r_v = Region("vaug")
    r_const = Region("const")
    r_small = Region("small"); r_comb = Region("combT")
    r_sm = [Region(f"sm{i}") for i in range(32)]
    W_B1 = [r_B1] + r_acc[0:4]
    W_B2 = [r_B2] + r_acc[4:8]

    scur = [0]

    def load_unit(src_ap, width=4096, view=None):
        i = scur[0] % NSLOT
        scur[0] += 1
        dst = s8(slots[i]) if view is None else view(slots[i])
        K.dma("pool", lambda e, d=dst, s=src_ap: e.dma_start(out=d, in_=s), reads=[], writes=[r_slot[i]])
        return slots[i], r_slot[i]

    def wview(W, c0, w):
        return W[:, c0:c0 + w].rearrange("(kc p) f -> p kc f", p=128)

    def s8(slot, w=512):
        return slot[:, 0:8 * w].rearrange("p (k f) -> p k f", k=8)

    def dump(name, ap2d, regs, dt):
        if not dbg:
            return
        t = nc.dram_tensor("dbg_" + name, list(ap2d.shape), dt, kind="ExternalOutput").ap()
        K.dma("sp", lambda e: e.dma_start(out=t[:, :], in_=ap2d), reads=list(regs), writes=[])

    def ld(dst, src, q="sp", regs=(r_const,)):
        K.dma(q, lambda e, d=dst, s=src: e.dma_start(out=d, in_=s), reads=[], writes=list(regs), parallel=True)

    r_c0 = Region("c_in")
    ld(CF, c_fm[:, :], regs=(r_c0,))
    ld(bfm[:, 0:48], b_ada_fm[:, :], regs=(r_const,))
    ld(N1, n1_fm[:, :], regs=(r_const,))
    ld(ident_b[:], k_ident[:, :], q="pool", regs=(r_const,))
    scur[0] = 0
    i_first = scur[0] % NSLOT
    K.dma("pool", lambda e: e.dma_start(out=s8(slots[i_first]), in_=wview(w_ada, 0, 512)), reads=[r_c0, r_const], writes=[r_slot[i_first]])
    scur[0] += 1
    ada_pre = [(slots[i_first], r_slot[i_first])] + [load_unit(wview(w_ada, 512 * u, 512)) for u in range(1, 4)]
    ld(N2, n2_fm[:, :]); ld(QG, qg_fm[:, :]); ld(KG, kg_fm[:, :])
    ld(GG, gg_fm[:, :]); ld(BG, bgate_fm[:, :])
    ld(ident_f[:], k_ident[:, :])
    ld(wr_sb[:], wr.rearrange("(kc p) j -> p kc j", p=128))
    ld(br_b[:], br_row.partition_broadcast(128))
    ld(esink[:], sinks_row.partition_broadcast(128))
    wsf, wsf_r = f32a.next()
    r2l_t, r2l_r = f32a.next()
    r2r_t, r2r_r = f32a.next()
    r2l = r2l_t[0:2, :].rearrange("p (g t) -> p g t", g=8)
    r2r = r2r_t[0:2, :]
    ld(r2l[0:1, :, :], gb_rows.rearrange("(o g) t -> o g t", o=1), regs=(r2l_r,))
    for g in range(8):
        ld(r2l[1:2, g, :], k_ones[0:1, :], regs=(r2l_r,))
    ld(r2r[1:2, :], bsp_row[0:1, :], regs=(r2r_r,))
    ld(wsf[:], wsT[:, :], regs=(wsf_r,))
    mkt, mkt_r = f32h.next()
    ld(mkt[:, 0:128], k_maskT[:, :], regs=(mkt_r,))
    ld(bones[:], k_bones[:, :], q="pool")
    ld(ones_bf[:], k_ones[0:1, :].rearrange("o p -> p o"), q="pool")
    ld(etab[:, 0:2048], k_etab[:, 0:2048], q="pool")
    ld(etab[:, 2048:4096], k_etab[:, 2048:4096], q="pool")
    ld(selb[:].rearrange("k e m -> k (e m)"), k_sel[:, :], q="pool")

    r_c = Region("c_act")
    K.op("act", lambda e: e.activation(out=CA, in_=CF, func=AF.Silu), reads=[r_c0], writes=[r_c0])
    K.op("dve", lambda e: e.tensor_copy(out=c_bf[:], in_=CA), reads=[r_c0], writes=[r_c])
    K.op("dve", lambda e: e.tensor_copy(out=c_rep[:], in_=CA.unsqueeze(2).to_broadcast([128, 8, 128])),
         reads=[r_c0], writes=[r_c])
    K.op("act", lambda e: e.activation(out=esink[:], in_=esink[:], func=AF.Exp), reads=[r_const], writes=[r_const])
    K.op("dve", lambda e: e.tensor_tensor(out=WmT[:], in0=wsf[:].rearrange("p (g t) -> p g t", g=8),
                                          in1=mkt[:, 0:128].unsqueeze(1).to_broadcast([128, 8, 128]), op=ALU.mult),
         reads=[wsf_r, mkt_r], writes=[r_const])
    K.op("dve", lambda e: e.memset(vaug[:], 1.0), reads=[], writes=[r_v])
    K.op("dve", lambda e: e.memset(small[:, 120:121], -0.5), reads=[], writes=[r_const])
    K.op("dve", lambda e: e.memset(kT[:], 0.0), reads=[], writes=[r_kT])

    b0, br_ = nb(2)
    for hh in range(2):
        K.op("pe", lambda e, hh=hh: e.matmul(PS[0:1, (b0 + hh) * 512:(b0 + hh + 1) * 512], ones_bf[:, 0:1],
                                            WmT[:, 4 * hh:4 * hh + 4, :], start=True, stop=True),
             reads=[r_const], writes=[br_[hh]])
    K.op("act", lambda e: e.activation(out=r2r[0:1, :], in_=PS[0:1, b0 * 512:(b0 + 2) * 512], func=AF.Copy),
         reads=br_, writes=[r2r_r])
    b0, br_ = nb(2)
    for g in range(8):
        K.op("pe", lambda e, g=g: e.matmul(PS[:, b0 * 512 + g * 128: b0 * 512 + (g + 1) * 128], r2l[0:2, g, :],
                                          r2r[0:2, g * 128:(g + 1) * 128], start=True, stop=True),
             reads=[r_const, r2l_r, r2r_r], writes=[br_[g // 4]], inc=(g % 4 == 3))
    K.op("act", lambda e: e.activation(out=R2[:].rearrange("p g t -> p (g t)"), in_=PS[:, b0 * 512:(b0 + 2) * 512],
                                       func=AF.Copy), reads=br_, writes=[r_const])

    r_g1 = Region("g1b"); r_a2 = Region("a2s2"); r_g2 = Region("g2b"); r_mod = Region("modfm")

    def ada_fm(units, lo_, hi_, pre=None):
        mb, mbr = nb(1)
        for n_, u in enumerate(units):
            slot, sr = pre[n_] if pre is not None else load_unit(wview(w_ada, 512 * u, 512))
            w8 = s8(slot)
            for fl in range(4):
                j = 4 * u + fl - lo_
                for kc in range(8):
                    K.op("pe", lambda e, kc=kc: e.matmul(
                        bank(mb, 1, j), w8[:, kc, fl * 128:(fl + 1) * 128], c_bf[:, kc:kc + 1],
                        start=(kc == 0), stop=(kc == 7)),
                         reads=[sr, r_c], writes=mbr, inc=(kc == 7 and fl == 3))
        K.op("dve", lambda e: e.tensor_tensor(out=modfm[:, lo_:hi_], in0=bank(mb, hi_ - lo_), in1=bfm[:, lo_:hi_], op=ALU.add),
             reads=mbr + [r_const], writes=[r_mod])

    def ada_rows(units, dst, dst_r):
        for n_, u in enumerate(units):
            slot, sr = load_unit(wview(w_ada, 512 * u, 512))
            w8 = s8(slot)
            gb, gbr = nb(1)
            for kc in range(8):
                K.op("pe", lambda e, kc=kc: e.matmul(bank(gb), c_rep[:, kc, :], w8[:, kc, :], start=(kc == 0), stop=(kc == 7)),
                     reads=[sr, r_c], writes=gbr, inc=(kc == 7))
            bt, bt_r = f32h.next()
            ld(bt[:], b_ada_row[0:1, 512 * u:512 * (u + 1)].partition_broadcast(128), regs=(bt_r,))
            K.op("dve", lambda e: e.tensor_tensor(out=dst[:, 512 * n_:512 * (n_ + 1)], in0=bank(gb), in1=bt[:], op=ALU.add),
                 reads=gbr + [bt_r], writes=[dst_r])

    ada_fm([0, 1, 2, 3], 0, 16, pre=ada_pre)
    K.op("dve", lambda e: e.scalar_tensor_tensor(out=A1, in0=modfm[:, 8:16], scalar=1.0, in1=N1, op0=ALU.add, op1=ALU.mult),
         reads=[r_const, r_mod], writes=[r_const])
    K.op("dve", lambda e: e.tensor_copy(out=S1, in_=modfm[:, 0:8]), reads=[r_mod], writes=[r_const])

    def ada_late_1():
        ada_rows([4, 5], g1b, r_g1)

    def ada_late_2():
        ada_fm([6, 7, 8, 9], 24, 40)
        K.op("dve", lambda e: e.scalar_tensor_tensor(out=A2, in0=modfm[:, 32:40], scalar=1.0, in1=N2, op0=ALU.add, op1=ALU.mult),
             reads=[r_const, r_mod], writes=[r_a2])
        K.op("dve", lambda e: e.tensor_copy(out=S2, in_=modfm[:, 24:32]), reads=[r_mod], writes=[r_a2])
        ada_rows([10, 11], g2b, r_g2)

    dump("modfm", modfm[:], [r_mod], F32)
    dump("R2", R2[:].rearrange("p g t -> p (g t)"), [r_const], F32)
    dump("cst", cst[:], [r_const], F32)

    def stop_if(name):
        if upto == name:
            raise _Stop()

    def bc8(v):
        return v.unsqueeze(2).to_broadcast([128, 8, 128])

    def rstd_pool(src_ap, src_regs, col, junk_ap, junk_r):
        ss = small[:, col:col + 1]
        rr = r_sm[col]
        K.op("act", lambda e: e.activation(out=junk_ap, in_=src_ap, func=AF.Square, accum_out=ss),
             reads=list(src_regs), writes=[junk_r, rr])
        K.op("dve", lambda e: e.tensor_scalar(out=ss, in0=ss, scalar1=1.0 / D, scalar2=EPS, op0=ALU.mult, op1=ALU.add),
             reads=[rr], writes=[rr])
        K.op("pool", lambda e: e.tensor_tensor(out=ss, in0=ss, in1=small[:, 120:121], op=ALU.pow),
             reads=[rr, r_const], writes=[rr])
        return ss, rr

    def rstd_of(src_ap, src_regs, col, junk_ap, junk_r):
        ss = small[:, col:col + 1]
        rr = r_sm[col]
        K.op("act", lambda e: e.activation(out=junk_ap, in_=src_ap, func=AF.Square, accum_out=ss),
             reads=list(src_regs), writes=[junk_r, rr])
        K.op("act", lambda e: e.activation(out=ss, in_=ss, func=AF.Ln, scale=1.0 / D, bias=EPS),
             reads=[rr], writes=[rr])
        K.op("act", lambda e: e.activation(out=ss, in_=ss, func=AF.Exp, scale=-0.5),
             reads=[rr], writes=[rr])
        return ss, rr

    def stage_A_act(st, blk):
        t0 = st * STT
        xin, xin_r = f32a.next()
        K.dma("sp", lambda e: e.dma_start(out=xin[:], in_=x[t0 + blk * 128: t0 + (blk + 1) * 128, :]), reads=[], writes=[xin_r])
        xs, xs_r = xsb.next()
        rs, rs_r = rstd_pool(xin[:], [xin_r], blk % 8, xs[:], xs_r)
        K.op("act", lambda e: e.activation(out=xs[:], in_=xin[:], func=AF.Copy, scale=rs), reads=[xin_r, rs_r], writes=[xs_r])
        return xs, xs_r

    def stage_A_pe(st, blk, xs, xs_r):
        pb, pbr = nb(1)
        for kc in range(8):
            K.op("pe", lambda e, kc=kc: e.transpose(out=bank_bf(pb)[:, kc * 128:(kc + 1) * 128],
                                                    in_=xs[:, kc * 128:(kc + 1) * 128], identity=ident_b[:]),
                 reads=[xs_r, r_const], writes=pbr, inc=(kc == 7))
        for kc in range(8):
            K.op("dve", lambda e, kc=kc: e.tensor_scalar(
                out=hT[:, kc, blk * 128:(blk + 1) * 128], in0=bank_bf(pb)[:, kc * 128:(kc + 1) * 128],
                scalar1=A1[:, kc:kc + 1], scalar2=S1[:, kc:kc + 1], op0=ALU.mult, op1=ALU.add),
                 reads=pbr + [r_const], writes=[r_hT[blk]])

    def stage_A(st):
        prev = None
        for blk in range(NBLK):
            cur = stage_A_act(st, blk)
            if prev is not None:
                stage_A_pe(st, blk - 1, *prev)
            prev = cur
        stage_A_pe(st, NBLK - 1, *prev)

    def body(st):
        t0 = st * STT
        stop_if("setup")
        if st == 0 or NOHOIST:
            stage_A(st)
        stop_if("A")
        if st == 0:
            dump("hT", hT[:].rearrange("p k t -> p (k t)"), r_hT, BF16)
        for j in range(2):
            slot, sr = load_unit(wview(w_in, 1536 + 512 * j, 512))
            w8 = s8(slot)
            for c in range(2):
                for fl in range(4):
                    pb, pbr = nb(1)
                    for kc in range(8):
                        K.op("pe", lambda e, kc=kc, w8=w8, fl=fl, c=c, pb=pb: e.matmul(
                            bank(pb), w8[:, kc, fl * 128:(fl + 1) * 128], hT[:, kc, c * 512:(c + 1) * 512],
                            start=(kc == 0), stop=(kc == 7)),
                             reads=[sr] + r_hT[4 * c:4 * c + 4], writes=pbr, inc=(kc == 7))
                    K.op("act", lambda e, pb=pb, j=j, fl=fl, c=c: e.activation(
                        out=B1[:, 4 * j + fl, c * 512:(c + 1) * 512], in_=bank(pb), func=AF.Gelu),
                         reads=pbr, writes=W_B1)
        vs = [load_unit(wview(w_in, 2560 + 512 * j, 512)) for j in range(2)]
        qs_pre = [load_unit(wview(w_in, 512 * j, 512)) for j in range(2)]
        i_k = scur[0] % NSLOT
        scur[0] += 1
        kslot, ksr = slots[i_k], r_slot[i_k]
        for dup in range(2):
            for kv in range(4):
                c0 = kv * 128 + dup * 64
                dstv = s8(kslot)[:, :, c0:c0 + 64]
                srcv = wview(w_in, 1024 + kv * 64, 64)
                K.dma("pool", lambda e, d=dstv, s=srcv: e.dma_start(out=d, in_=s), reads=[], writes=[ksr], parallel=(dup + kv > 0))
        v_pre = load_unit(wview(w_in, 1280, 256), width=2048, view=lambda s_: s_[:, 0:2048].rearrange("p (k f) -> p k f", k=8))
        def vg_block(blk):
            gv, gv_r = f32a.next()
            for j in range(2):
                pb, pbr = nb(1)
                w8 = s8(vs[j][0])
                for kc in range(8):
                    K.op("pe", lambda e, kc=kc: e.matmul(
                        bank(pb), hT[:, kc, blk * 128:(blk + 1) * 128], w8[:, kc, :], start=(kc == 0), stop=(kc == 7)),
                         reads=[vs[j][1], r_hT[blk]], writes=pbr, inc=(kc == 7))
                K.op("act", lambda e: e.activation(out=gv[:, 512 * j:512 * (j + 1)], in_=bank(pb), func=AF.Gelu),
                     reads=pbr, writes=[gv_r])
            so_ = 16 + 16 * (blk % 4)
            lr = r_sm[16 + blk % 4]
            for j in range(2):
                K.op("dve", lambda e, j=j: e.bn_stats(out=small[:, so_ + 6 * j:so_ + 6 + 6 * j], in_=gv[:, 512 * j:512 * (j + 1)]),
                     reads=[gv_r], writes=[lr])
            K.op("dve", lambda e: e.bn_aggr(out=small[:, so_ + 12:so_ + 14], in_=small[:, so_:so_ + 12]), reads=[lr], writes=[lr])
            K.op("dve", lambda e: e.tensor_scalar(out=small[:, so_ + 14:so_ + 15], in0=small[:, so_ + 13:so_ + 14],
                                                  scalar1=EPS, scalar2=None, op0=ALU.add), reads=[lr], writes=[lr])
            K.op("pool", lambda e: e.tensor_tensor(out=small[:, so_ + 15:so_ + 16], in0=small[:, so_ + 14:so_ + 15],
                                                   in1=small[:, 120:121], op=ALU.pow), reads=[lr, r_const], writes=[lr])
            K.op("dve", lambda e: e.tensor_scalar(
                out=B2[:, blk, :], in0=gv[:], scalar1=small[:, so_ + 12:so_ + 13], scalar2=small[:, so_ + 15:so_ + 16],
                op0=ALU.subtract, op1=ALU.mult), reads=[gv_r, lr], writes=W_B2)
            return gv, gv_r

        def sp_block(blk, tmp, tmp_r):
            b0, b0r = nb(2)
            for g in range(8):
                K.op("pe", lambda e, g=g: e.matmul(
                    PS[:, b0 * 512 + g * 128: b0 * 512 + (g + 1) * 128], B2[:, blk, g * 128:(g + 1) * 128], WmT[:, g, :],
                    start=True, stop=True), reads=[r_B2, r_const], writes=[b0r[g // 4]], inc=(g % 4 == 3))
            t3 = tmp[:].rearrange("p (g t) -> p g t", g=8)
            K.op("dve", lambda e: e.tensor_tensor(
                out=t3, in0=PS[:, b0 * 512:(b0 + 2) * 512].rearrange("p (g t) -> p g t", g=8), in1=bc8(GG), op=ALU.mult),
                 reads=b0r + [r_const], writes=[tmp_r])
            K.op("dve", lambda e: e.tensor_tensor(out=t3, in0=t3, in1=R2[:], op=ALU.add), reads=[tmp_r, r_const], writes=[tmp_r])
            K.op("dve", lambda e: e.tensor_tensor(
                out=B1[:, :, blk * 128:(blk + 1) * 128], in0=t3, in1=B1[:, :, blk * 128:(blk + 1) * 128], op=ALU.mult),
                 reads=[tmp_r, r_B1], writes=W_B1)

        LOOK = 2
        gvs = {}
        for blk in range(NBLK + LOOK):
            if blk < NBLK:
                gvs[blk] = vg_block(blk)
            if blk - LOOK >= 0:
                sp_block(blk - LOOK, *gvs.pop(blk - LOOK))
        stop_if("B1")

        if st == 0:
            dump("ygT_", BB[:, 0:8192], [r_B1], BF16) if False else None
        stop_if("B")
        if st == 0:
            dump("ygT", BB[:, 0:8192], [r_B1], BF16)
        def qk_norm(pb, pbr, gain, dst_ap, dst_regs):
            sq, sq_r = bfh.next()
            K.op("act", lambda e: e.activation(out=sq[:], in_=bank(pb), func=AF.Square), reads=pbr, writes=[sq_r])
            p2, p2r = nb(1)
            K.op("pe", lambda e: e.matmul(bank(p2), bones[:], sq[:], start=True, stop=True),
                 reads=[sq_r, r_const], writes=p2r)
            rq, rq_r = f32h.next()
            K.op("act", lambda e: e.activation(out=rq[:], in_=bank(p2), func=AF.Ln, bias=64.0 * EPS), reads=p2r, writes=[rq_r])
            K.op("act", lambda e: e.activation(out=rq[:], in_=rq[:], func=AF.Exp, scale=-0.5), reads=[rq_r], writes=[rq_r])
            K.op("dve", lambda e: e.scalar_tensor_tensor(out=dst_ap, in0=bank(pb), scalar=gain, in1=rq[:],
                                                         op0=ALU.mult, op1=ALU.mult),
                 reads=pbr + [rq_r, r_const], writes=dst_regs)

        qk_pending = []

        def qk_task(w8, col, c, sr, gain, dst, dst_regs):
            pb, pbr = nb(1)
            for kc in range(8):
                K.op("pe", lambda e, kc=kc: e.matmul(
                    bank(pb), w8[:, kc, col:col + 128], hT[:, kc, c * 512:(c + 1) * 512],
                    start=(kc == 0), stop=(kc == 7)),
                     reads=[sr] + r_hT[4 * c:4 * c + 4], writes=pbr, inc=(kc == 7))
            if qk_pending:
                qk_norm(*qk_pending.pop())
            qk_pending.append((pb, pbr, gain, dst, dst_regs))

        for j in range(2):
            slot, sr = qs_pre[j]
            w8 = s8(slot)
            for c in range(2):
                for fl in range(4):
                    qk_task(w8, fl * 128, c, sr, QG, B3[:, 4 * j + fl, c * 512:(c + 1) * 512], [r_B3])
        w8 = s8(kslot)
        for c in range(2):
            for kv in range(4):
                qk_task(w8, kv * 128, c, ksr, KG, kT[:, kv, 128 + c * 512:128 + (c + 1) * 512], [r_kT])
        qk_norm(*qk_pending.pop())
        slot, sr = v_pre
        wv8 = slot[:, 0:2048].rearrange("p (k f) -> p k f", k=8)
        for blk in range(NBLK):
            pb, pbr = nb(1)
            for kc in range(8):
                K.op("pe", lambda e, kc=kc, blk=blk, pb=pb: e.matmul(
                    bank(pb, 256), hT[:, kc, blk * 128:(blk + 1) * 128], wv8[:, kc, :], start=(kc == 0), stop=(kc == 7)),
                     reads=[sr, r_hT[blk]], writes=pbr, inc=(kc == 7))
            K.op("act", lambda e, blk=blk, pb=pb: e.activation(
                out=vaug[:, 1 + blk, :, 0:64], in_=bank(pb, 256).rearrange("p (v d) -> p v d", v=4), func=AF.Copy),
                 reads=pbr, writes=[r_v])
        stop_if("C1")
        if st == 0:
            dump("qT", B3[:].rearrange("p k t -> p (k t)"), [r_B3], BF16)
            dump("kT", kT[:].rearrange("p k t -> p (k t)"), [r_kT], BF16)
            dump("vaug", vaug[:].rearrange("p b v d -> p (b v d)"), [r_v], BF16)
        def att_qk(blk, g, js):
            pts = {}
            lo = 256 if len(js) == 1 else 0
            for i in range(2):
                pb, pbr = nbhi()
                for jj in js:
                    koff = blk * 128 + jj * 128
                    K.op("pe", lambda e, jj=jj, koff=koff: e.matmul(
                        bank(pb, 256, 256 * jj).rearrange("p (c q) -> p c q", c=2),
                        kT[64 * i:64 * i + 64, g, koff:koff + 128],
                        B3[64 * i:64 * i + 64, 2 * g:2 * g + 2, blk * 128:(blk + 1) * 128],
                        start=True, stop=True), reads=[r_kT, r_B3], writes=pbr, inc=(jj == 1))
                pe_, pe_r = bfh.next()
                K.op("act", lambda e: e.activation(out=pe_[:, lo:512], in_=bank(pb, 512 - lo, lo), func=AF.Exp, scale=8.0),
                     reads=pbr, writes=[pe_r])
                pt, pt_r = ptp.next()
                eo = (g * 2 + i) * 512
                K.op("dve", lambda e: e.tensor_tensor(out=pt[:, lo:512], in0=pe_[:, lo:512], in1=etab[:, eo + lo:eo + 512], op=ALU.mult),
                     reads=[pe_r, r_const], writes=[pt_r])
                pts[i] = (pt, pt_r)
            return pts

        def att_pv(blk, g, js, pts, yps, ybr):
            for hl in range(4):
                h = 4 * g + hl
                i = h % 2
                cc = (h % 4) // 2
                pt, pt_r = pts[i]
                for n_, jj in enumerate(js):
                    col = jj * 256 + cc * 128
                    K.op("pe", lambda e, n_=n_, jj=jj, col=col: e.matmul(
                        yps[:, 4 * (g % 2) + hl, 0:65], pt[:, col:col + 128], vaug[:, blk + jj, g, 0:65],
                        start=(n_ == 0), stop=(n_ == len(js) - 1)),
                         reads=[pt_r, r_v], writes=[ybr[g % 2]], inc=(n_ == len(js) - 1 and hl == 3))

        def att_norm(blk, g, yps, ybr, yt, yt_r):
            k4 = (4 * blk + g) % 4
            den = small[:, 80 + 4 * k4:84 + 4 * k4]
            dr = r_sm[24 + k4]
            yg = yps[:, 4 * (g % 2):4 * (g % 2) + 4, :]
            K.op("dve", lambda e: e.tensor_tensor(out=den.unsqueeze(2), in0=yg[:, :, 64:65],
                                                  in1=esink[:, 4 * g:4 * g + 4].unsqueeze(2), op=ALU.add),
                 reads=[ybr[g % 2], r_const], writes=[dr])
            K.op("dve", lambda e: e.reciprocal(out=den, in_=den), reads=[dr], writes=[dr])
            K.op("dve", lambda e: e.tensor_tensor(
                out=yt[:, 256 * g:256 * (g + 1)].rearrange("p (h d) -> p h d", h=4), in0=yg[:, :, 0:64],
                in1=den.unsqueeze(2).to_broadcast([128, 4, 64]), op=ALU.mult),
                 reads=[ybr[g % 2], dr], writes=[yt_r])

        def att_fin(blk, yt, yt_r):
            pb, pbr = nbhi()
            for kc in range(8):
                K.op("pe", lambda e, kc=kc: e.transpose(out=bank_bf(pb)[:, kc * 128:(kc + 1) * 128],
                                                        in_=yt[:, kc * 128:(kc + 1) * 128], identity=ident_b[:]),
                     reads=[yt_r, r_const], writes=pbr, inc=(kc == 7))
            K.op("act", lambda e: e.activation(
                out=B2[:, :, blk * 128:(blk + 1) * 128], in_=bank_bf(pb).rearrange("p (k t) -> p k t", k=8), func=AF.Copy),
                 reads=pbr, writes=W_B2)

        ybr = bank_r[0:2]
        yps = PS[:, 0:1024].rearrange("p (h d) -> p h d", h=8)
        units = [(blk, g) for blk in range(NBLK) for g in range(4)]
        jsof = lambda blk: [1] if (st * NBLK + blk) == 0 else [0, 1]
        ALOOK = 2
        pend = {}
        yts = {}
        for n in range(len(units) + ALOOK):
            if n < len(units):
                b2, g2 = units[n]
                pend[n] = att_qk(b2, g2, jsof(b2))
            m_ = n - ALOOK
            if m_ >= 0:
                blk, g = units[m_]
                if g == 0:
                    yts[blk] = ytm.next()
                att_pv(blk, g, jsof(blk), pend.pop(m_), yps, ybr)
                att_norm(blk, g, yps, ybr, *yts[blk])
                if g == 3:
                    att_fin(blk, *yts.pop(blk))
        stop_if("C")
        if st == 0:
            dump("yaT", BB[:, 8192:16384], [r_B2], BF16)
        K.op("dve", lambda e: e.tensor_copy(out=kT[:, :, 0:128], in_=kT[:, :, 1024:1152]), reads=[r_kT], writes=[r_kT])
        K.op("dve", lambda e: e.tensor_copy(out=vaug[:, 0, :, :], in_=vaug[:, 8, :, :]), reads=[r_v], writes=[r_v])

        if st == 0:
            ada_late_1()
        for j in range(2):
            for br_i, (Wo, src, src_r, gcol) in enumerate(((w_oa, B2, r_B2, 3584), (w_og, B1, r_B1, 4608))):
                so, sor = load_unit(wview(Wo, 512 * j, 512))
                sg_, sgr = load_unit(wview(w_in, gcol + 512 * j, 512))
                wo8 = s8(so); wg8 = s8(sg_)
                for c in range(2):
                    for fl in range(4):
                        fch = 4 * j + fl
                        pa, par = nb(1)
                        for kc in range(8):
                            K.op("pe", lambda e, kc=kc, wo8=wo8, fl=fl, c=c, pa=pa, src=src: e.matmul(
                                bank(pa), wo8[:, kc, fl * 128:(fl + 1) * 128], src[:, kc, c * 512:(c + 1) * 512],
                                start=(kc == 0), stop=(kc == 7)), reads=[sor, src_r], writes=par, inc=(kc == 7))
                        pg, pgr = nb(1)
                        for kc in range(8):
                            K.op("pe", lambda e, kc=kc, wg8=wg8, fl=fl, c=c, pg=pg: e.matmul(
                                bank(pg), wg8[:, kc, fl * 128:(fl + 1) * 128], hT[:, kc, c * 512:(c + 1) * 512],
                                start=(kc == 0), stop=(kc == 7)), reads=[sgr] + r_hT[4 * c:4 * c + 4], writes=pgr, inc=(kc == 7))
                        sg, sg_r = f32h.next()
                        bcol = BG[:, 8 * br_i + fch: 8 * br_i + fch + 1]
                        K.op("act", lambda e, sg=sg, pg=pg, bcol=bcol: e.activation(out=sg[:], in_=bank(pg), func=AF.Sigmoid, bias=bcol),
                             reads=pgr + [r_const], writes=[sg_r])
                        dst = B3[:, fch, c * 512:(c + 1) * 512]
                        if br_i == 0:
                            K.op("dve", lambda e, sg=sg, pa=pa, dst=dst: e.tensor_tensor(out=dst, in0=bank(pa), in1=sg[:], op=ALU.mult),
                                 reads=par + [sg_r], writes=[r_B3])
                        else:
                            t2, t2_r = f32h.next()
                            K.op("dve", lambda e, sg=sg, pa=pa, t2=t2: e.tensor_tensor(out=t2[:], in0=bank(pa), in1=sg[:], op=ALU.mult),
                                 reads=par + [sg_r], writes=[t2_r])
                            K.op("dve", lambda e, t2=t2, dst=dst: e.tensor_tensor(out=dst, in0=t2[:], in1=dst, op=ALU.add),
                                 reads=[t2_r, r_B3], writes=[r_B3])

        stop_if("D")
        if st == 0:
            dump("mergedT", B3[:].rearrange("p k t -> p (k t)"), [r_B3], BF16)
        if st == 0:
            ada_late_2()
        for j in range(2):
            slot, sr = load_unit(wview(w_out, 512 * j, 512))
            w8 = s8(slot)
            K.op("dve", lambda e, w8=w8, j=j: e.tensor_tensor(
                out=w8, in0=w8, in1=g1b[:, 512 * j:512 * (j + 1)].unsqueeze(1).to_broadcast([128, 8, 512]), op=ALU.mult),
                 reads=[sr, r_g1], writes=[sr])
            for blk in range(NBLK):
                xh, xh_r = f32h.next()
                K.dma("sp", lambda e, xh=xh, blk=blk, j=j: e.dma_start(
                    out=xh[:], in_=x[t0 + blk * 128: t0 + (blk + 1) * 128, 512 * j:512 * (j + 1)]), reads=[], writes=[xh_r])
                pb, pbr = nb(1)
                for kc in range(8):
                    K.op("pe", lambda e, kc=kc, w8=w8, blk=blk, pb=pb: e.matmul(
                        bank(pb), B3[:, kc, blk * 128:(blk + 1) * 128], w8[:, kc, :], start=(kc == 0), stop=(kc == 7)),
                         reads=[sr, r_B3], writes=pbr, inc=(kc == 7))
                K.op("dve", lambda e, xh=xh, pb=pb, blk=blk, j=j: e.tensor_tensor(
                    out=acc[:, blk, 512 * j:512 * (j + 1)], in0=bank(pb), in1=xh[:], op=ALU.add),
                     reads=pbr + [xh_r], writes=[r_acc[blk], r_B1 if blk < 4 else r_B2])

        stop_if("E")
        if st == 0:
            dump("xn", BB[:].bitcast(F32), r_acc, F32)
        rt, rt_r = rt_tile, r_rt
        GL = rt[:, 0:32].rearrange("p (b g) -> p b g", b=8)
        EL = rt[:, 32:160]
        def f_head(blk):
            xs32, xs32_r = f32a.next()
            rs, rs_r = rstd_of(acc[:, blk, :], [r_acc[blk]], 8 + blk % 8, xs32[:], xs32_r)
            K.op("act", lambda e: e.activation(out=xs32[:], in_=acc[:, blk, :], func=AF.Copy, scale=rs),
                 reads=[r_acc[blk], rs_r], writes=[xs32_r])
            b0, b0r = nb(2)
            for kc in range(8):
                K.op("pe", lambda e, kc=kc: e.transpose(
                    out=PS[:, b0 * 512 + kc * 128: b0 * 512 + (kc + 1) * 128], in_=xs32[:, kc * 128:(kc + 1) * 128],
                    identity=ident_f[:]), reads=[xs32_r, r_const], writes=[b0r[kc // 4]], inc=(kc % 4 == 3))
            h2f, h2f_r = xs32, xs32_r
            h3 = h2f[:].rearrange("p (k t) -> p k t", k=8)
            K.op("dve", lambda e: e.tensor_tensor(
                out=h3, in0=PS[:, b0 * 512:(b0 + 2) * 512].rearrange("p (k t) -> p k t", k=8), in1=bc8(A2), op=ALU.mult),
                 reads=b0r + [r_a2], writes=[h2f_r])
            K.op("dve", lambda e: e.tensor_tensor(out=h3, in0=h3, in1=bc8(S2), op=ALU.add),
                 reads=[h2f_r, r_a2], writes=[h2f_r])
            return h3, h2f_r

        def f_tail(blk, h3, h2f_r):
            K.op("act", lambda e: e.activation(out=B3[:, :, blk * 128:(blk + 1) * 128], in_=h3, func=AF.Copy),
                 reads=[h2f_r], writes=[r_B3])
            pb, pbr = nb(1)
            for kc in range(8):
                K.op("pe", lambda e, kc=kc: e.matmul(bank(pb, 20), h3[:, kc, :], wr_sb[:, kc, :], start=(kc == 0), stop=(kc == 7)),
                     reads=[h2f_r, r_const], writes=pbr, inc=(kc == 7))
            K.op("dve", lambda e: e.tensor_tensor(out=GL[:, blk, :], in0=bank(pb, 4), in1=br_b[:, 0:4], op=ALU.add),
                 reads=pbr + [r_const], writes=[rt_r])
            K.op("dve", lambda e: e.tensor_tensor(out=EL[:, 16 * blk:16 * blk + 16], in0=bank(pb, 16, 4), in1=br_b[:, 4:20], op=ALU.add),
                 reads=pbr + [r_const], writes=[rt_r])

        prev = None
        for blk in range(NBLK):
            cur = f_head(blk)
            if prev is not None:
                f_tail(blk - 1, *prev)
            prev = cur
        f_tail(NBLK - 1, *prev)
        o = 160

        def v8(n):
            nonlocal o
            a = rt[:, o:o + 8 * n].rearrange("p (b n) -> p b n", b=8)
            o += 8 * n
            return a

        ngm = v8(1); T4 = v8(4); goh = v8(4); gex = v8(4); gsum = v8(1); gw = v8(1)
        TM = v8(16); esel = v8(4); nm1 = v8(1); T5 = v8(4); oh1 = v8(4); e2 = v8(4); nm2 = v8(1); oh2 = v8(4)
        dd = v8(1); ed = v8(1); w1 = v8(1); w2 = v8(1); ta = v8(4); wig = v8(4); comb = v8(16)
        RS = [rt_r]

        def sop(fn, eng="dve"):
            K.op(eng, fn, reads=RS, writes=RS)

        def b4(a):
            return a.to_broadcast([128, 8, 4])

        sop(lambda e: e.tensor_reduce(out=ngm, in_=GL, axis=AX.X, op=ALU.max, negate=True))
        sop(lambda e: e.tensor_tensor(out=T4, in0=GL, in1=b4(ngm), op=ALU.add))
        sop(lambda e: e.tensor_single_scalar(out=goh, in_=T4, scalar=0.0, op=ALU.is_ge))
        sop(lambda e: e.activation(out=gex, in_=T4, func=AF.Exp), eng="act")
        sop(lambda e: e.tensor_reduce(out=gsum, in_=gex, axis=AX.X, op=ALU.add))
        sop(lambda e: e.reciprocal(out=gw, in_=gsum))
        for g in range(4):
            sop(lambda e, g=g: e.tensor_tensor(out=TM[:, :, 4 * g:4 * g + 4],
                                               in0=EL.rearrange("p (b n) -> p b n", b=8)[:, :, 4 * g:4 * g + 4],
                                               in1=b4(goh[:, :, g:g + 1]), op=ALU.mult))
        sop(lambda e: e.tensor_tensor(out=esel, in0=TM[:, :, 0:4], in1=TM[:, :, 4:8], op=ALU.add))
        sop(lambda e: e.tensor_tensor(out=esel, in0=esel, in1=TM[:, :, 8:12], op=ALU.add))
        sop(lambda e: e.tensor_tensor(out=esel, in0=esel, in1=TM[:, :, 12:16], op=ALU.add))
        sop(lambda e: e.tensor_reduce(out=nm1, in_=esel, axis=AX.X, op=ALU.max, negate=True))
        sop(lambda e: e.tensor_tensor(out=T5, in0=esel, in1=b4(nm1), op=ALU.add))
        sop(lambda e: e.tensor_single_scalar(out=oh1, in_=T5, scalar=0.0, op=ALU.is_ge))
        sop(lambda e: e.scalar_tensor_tensor(out=e2, in0=oh1, scalar=-1e30, in1=esel, op0=ALU.mult, op1=ALU.add))
        sop(lambda e: e.tensor_reduce(out=nm2, in_=e2, axis=AX.X, op=ALU.max, negate=True))
        sop(lambda e: e.tensor_tensor(out=T5, in0=e2, in1=b4(nm2), op=ALU.add))
        sop(lambda e: e.tensor_single_scalar(out=oh2, in_=T5, scalar=0.0, op=ALU.is_ge))
        sop(lambda e: e.tensor_tensor(out=dd, in0=nm1, in1=nm2, op=ALU.subtract))
        sop(lambda e: e.activation(out=ed, in_=dd, func=AF.Exp), eng="act")
        sop(lambda e: e.tensor_single_scalar(out=w1, in_=ed, scalar=1.0, op=ALU.add))
        sop(lambda e: e.reciprocal(out=w1, in_=w1))
        sop(lambda e: e.tensor_tensor(out=w1, in0=w1, in1=gw, op=ALU.mult))
        sop(lambda e: e.tensor_tensor(out=w2, in0=w1, in1=ed, op=ALU.mult))
        sop(lambda e: e.tensor_tensor(out=ta, in0=oh1, in1=b4(w1), op=ALU.mult))
        sop(lambda e: e.tensor_tensor(out=wig, in0=oh2, in1=b4(w2), op=ALU.mult))
        sop(lambda e: e.tensor_tensor(out=wig, in0=wig, in1=ta, op=ALU.add))
        for g in range(4):
            sop(lambda e, g=g: e.tensor_tensor(out=comb[:, :, 4 * g:4 * g + 4], in0=wig, in1=b4(goh[:, :, g:g + 1]), op=ALU.mult))
        for blk in range(NBLK):
            pc, pcr = nb(1)
            K.op("pe", lambda e, pc=pc, blk=blk: e.transpose(out=PS[0:16, pc * 512: pc * 512 + 128], in_=comb[:, blk, :],
                                                            identity=ident_f[:]),
                 reads=RS + [r_const], writes=pcr)
            K.op("act", lambda e, pc=pc, blk=blk: e.activation(out=combT[0:16, blk * 128:(blk + 1) * 128],
                                                              in_=PS[0:16, pc * 512: pc * 512 + 128], func=AF.Copy),
                 reads=pcr, writes=[r_comb])

        stop_if("F")
        if st == 0:
            dump("h2T", B3[:].rearrange("p k t -> p (k t)"), [r_B3], BF16)
            dump("combT", combT[:], [r_comb], BF16)
        def moe_gu(ex, c, wg8, wu8, sgr, sur):
            hid, hid_r = hidp.next()
            cb, cb_r = bfh.next()
            for fl in range(4):
                pg, pgr = nb(1)
                for kc in range(8):
                    K.op("pe", lambda e, kc=kc: e.matmul(
                        bank(pg), wg8[:, kc, fl * 128:(fl + 1) * 128], B3[:, kc, c * 512:(c + 1) * 512],
                        start=(kc == 0), stop=(kc == 7)), reads=[sgr, r_B3], writes=pgr, inc=(kc == 7))
                pu, pur = nb(1)
                for kc in range(8):
                    K.op("pe", lambda e, kc=kc: e.matmul(
                        bank(pu), wu8[:, kc, fl * 128:(fl + 1) * 128], B3[:, kc, c * 512:(c + 1) * 512],
                        start=(kc == 0), stop=(kc == 7)), reads=[sur, r_B3], writes=pur, inc=(kc == 7))
                if fl == 0:
                    pcb, pcbr = nb(1)
                    K.op("pe", lambda e: e.matmul(bank(pcb), selb[0:16, ex, :], combT[0:16, c * 512:(c + 1) * 512],
                                                  start=True, stop=True), reads=[r_comb, r_const], writes=pcbr)
                    K.op("act", lambda e: e.activation(out=cb[:], in_=bank(pcb), func=AF.Copy), reads=pcbr, writes=[cb_r])
                sl, sl_r = f32h.next()
                K.op("act", lambda e: e.activation(out=sl[:], in_=bank(pg), func=AF.Silu), reads=pgr, writes=[sl_r])
                K.op("dve", lambda e: e.tensor_tensor(out=sl[:], in0=bank(pu), in1=sl[:], op=ALU.mult),
                     reads=pur + [sl_r], writes=[sl_r])
                K.op("dve", lambda e: e.tensor_tensor(out=hid[:, fl, :], in0=sl[:], in1=cb[:], op=ALU.mult),
                     reads=[sl_r, cb_r], writes=[hid_r])
            return hid, hid_r

        def moe_dn(ex, c, hid, hid_r, wd4, sdr):
            for tt in range(4):
                blk = 4 * c + tt
                for dh in range(2):
                    pd, pdr = nb(1)
                    for fl in range(4):
                        K.op("pe", lambda e, fl=fl: e.matmul(
                            bank(pd), hid[:, fl, tt * 128:(tt + 1) * 128], wd4[:, fl, dh * 512:(dh + 1) * 512],
                            start=(fl == 0), stop=(fl == 3)), reads=[hid_r, sdr], writes=pdr, inc=(fl == 3))
                    K.op("dve", lambda e: e.tensor_tensor(
                        out=acc[:, blk, 512 * dh:512 * (dh + 1)], in0=bank(pd), in1=acc[:, blk, 512 * dh:512 * (dh + 1)], op=ALU.add),
                         reads=pdr + [r_acc[blk]], writes=[r_acc[blk]])
                if ex == 15:
                    K.dma("sp", lambda e: e.dma_start(out=out[t0 + blk * 128: t0 + (blk + 1) * 128, :], in_=acc[:, blk, :]),
                          reads=[r_acc[blk]], writes=[])

        for ex in range(16):
            sg_, sgr = load_unit(w_eg[ex].rearrange("(kc p) f -> p kc f", p=128))
            su_, sur = load_unit(w_eu[ex].rearrange("(kc p) f -> p kc f", p=128))
            sd_, sdr = load_unit(w_ed[ex].rearrange("(fc p) d -> p fc d", p=128),
                                 view=lambda s_: s_[:].rearrange("p (k f) -> p k f", k=4))
            wg8 = s8(sg_); wu8 = s8(su_)
            wd4 = sd_[:].rearrange("p (k f) -> p k f", k=4)
            hoist = (ex >= 8 and st + 1 < n_st and upto is None and not NOHOIST)
            if hoist:
                a_nxt = stage_A_act(st + 1, ex - 8)
            h0 = moe_gu(ex, 0, wg8, wu8, sgr, sur)
            K.op("dve", lambda e: e.tensor_tensor(out=wd4, in0=wd4, in1=g2b[:].unsqueeze(1).to_broadcast([128, 4, 1024]),
                                                  op=ALU.mult), reads=[sdr, r_g2], writes=[sdr])
            h1 = moe_gu(ex, 1, wg8, wu8, sgr, sur)
            moe_dn(ex, 0, h0[0], h0[1], wd4, sdr)
            if hoist:
                stage_A_pe(st + 1, ex - 8, *a_nxt)
            moe_dn(ex, 1, h1[0], h1[1], wd4, sdr)

    try:
        for st_ in range(n_st):
            body(st_)
    except _Stop:
        pass
    K.finish("sp")
    sems = {k: es.enter_context(nc.semaphore(k)) for k in K.sem_names()}
    block = es.enter_context(nc.Block())
    K.replay(nc, block, sems)
    es.close()
    return nc


def host_inputs(inp, b):
    f = lambda a: np.ascontiguousarray(a, dtype=np.float32)
    fm = lambda v: f(np.asarray(v).reshape(-1, 128).T)
    m = {}
    m["x"] = f(inp["x"][b])
    m["c_fm"] = fm(inp["c"][b])
    m["w_ada"] = f(inp["w_ada"][0])
    m["b_ada_fm"] = fm(inp["b_ada"][0])
    m["b_ada_row"] = f(inp["b_ada"][0][None, :])
    m["n1_fm"] = fm(inp["norm1_gain"][0])
    m["n2_fm"] = fm(inp["norm2_gain"][0])
    m["w_in"] = f(inp["w_in"][0])
    m["bgate_fm"] = fm(inp["b_branch_gate"][0])
    m["qg_fm"] = f(np.tile(inp["q_norm_gain"][0], 2)[:, None])
    m["kg_fm"] = f(np.tile(inp["k_norm_gain"][0], 2)[:, None])
    m["sinks_row"] = f(inp["attn_sinks"][0][None, :])
    m["gg_fm"] = fm(inp["gmlp_norm_gain"][0])
    m["gb_rows"] = f(inp["gmlp_norm_bias"][0].reshape(8, 128))
    m["wsT"] = f(np.transpose(inp["gmlp_w_spatial"][0], (2, 0, 1)).reshape(128, 1024))
    m["bsp_row"] = f(inp["gmlp_b_spatial"][0].reshape(1, 1024))
    m["w_oa"] = f(inp["w_o_attn"][0])
    m["w_og"] = f(inp["w_o_gmlp"][0])
    m["w_out"] = f(inp["w_out"][0])
    m["wr"] = f(np.concatenate([inp["w_group_router"][0], inp["w_expert_router"][0]], axis=1))
    m["br_row"] = f(np.concatenate([inp["b_group_router"][0], inp["b_expert_router"][0]])[None, :])
    m["w_eg"] = f(inp["w_expert_gate"][0].reshape(16, 1024, 512))
    m["w_eu"] = f(inp["w_expert_up"][0].reshape(16, 1024, 512))
    m["w_ed"] = f(inp["w_expert_down"][0].reshape(16, 512, 1024))
    return m


def host_consts():
    m = {}
    m["k_ident"] = np.eye(128, dtype=np.float32)
    m["k_etab"] = alibi_tables()
    s_idx = np.arange(128)[:, None]
    t_idx = np.arange(128)[None, :]
    m["k_maskT"] = (s_idx <= t_idx).astype(np.float32)
    bo = np.zeros((128, 128), np.float32)
    bo[:64, :64] = 1.0
    bo[64:, 64:] = 1.0
    m["k_bones"] = bo
    sel = np.zeros((16, 16, 128), np.float32)
    for e_ in range(16):
        sel[e_, e_, :] = 1.0
    m["k_sel"] = sel.reshape(16, 2048)
    m["k_ones"] = np.ones((1, 128), np.float32)
    return m


_NC_CACHE = {}


def kernel(**inputs):
    inp = {k: np.asarray(v) for k, v in inputs.items()}
    if "nc" not in _NC_CACHE:
        _NC_CACHE["nc"] = build_nc()
    nc = _NC_CACHE["nc"]
    consts = host_consts()
    in_maps = []
    for b in range(8):
        m = host_inputs(inp, b)
        m.update(consts)
        in_maps.append(m)
    res = run_bass_kernel_spmd(nc, in_maps, core_ids=list(range(8)))
    return np.stack([np.asarray(r["out"]).reshape(S, D) for r in res.results], axis=0).astype(np.float32)
```

```python
import numpy as np
from contextlib import ExitStack
import concourse.bass as bass
import concourse.mybir as mybir
from concourse.bass_utils import run_bass_kernel_spmd

F32 = mybir.dt.float32
BF16 = mybir.dt.bfloat16
AF = mybir.ActivationFunctionType
ALU = mybir.AluOpType
AX = mybir.AxisListType

S = 4096
D = 1024
STT = 1024
NBLK = 8
NSLOT = 6
EPS = 1e-6
NOHOIST = False
HOIST_EX = 11
ENGS = ("pe", "act", "dve", "pool", "sp")


class Region:
    __slots__ = ("name", "last_w", "extra_w", "readers")

    def __init__(self, name):
        self.name = name
        self.last_w = None
        self.extra_w = []
        self.readers = {}


class Sync:
    def __init__(self, n_dma_sp=8, n_dma_pool=6):
        self.prog = {e: [] for e in ENGS}
        self.cnt = {e: 0 for e in ENGS}
        self.waited = {e: {} for e in ENGS}
        self.dma_sems = {"sp": [f"dsp{i}" for i in range(n_dma_sp)],
                         "pool": [f"dpl{i}" for i in range(n_dma_pool)]}
        self.dma_tot = {}
        for q in self.dma_sems:
            for k in self.dma_sems[q]:
                self.dma_tot[k] = 0
        self.dma_rr = {"sp": 0, "pool": 0}
        self.n_ops = 0

    def sem_names(self):
        return list(ENGS) + [k for q in self.dma_sems for k in self.dma_sems[q]]

    def _wait(self, e, ev):
        k, v = ev
        if self.waited[e].get(k, 0) >= v:
            return
        self.waited[e][k] = v
        self.prog[e].append(("wait", k, v))

    def _deps(self, e, reads, writes, parallel=False):
        deps = []
        for r in reads:
            if r.last_w is not None:
                deps.append(r.last_w)
            deps.extend(r.extra_w)
        for w in writes:
            if not parallel:
                if w.last_w is not None:
                    deps.append(w.last_w)
                deps.extend(w.extra_w)
            deps.extend(w.readers.items())
        for ev in deps:
            if ev[0] == e and e == "pe":
                continue
            self._wait(e, ev)

    def _commit(self, ev, reads, writes, parallel=False):
        k, v = ev
        for r in reads:
            if r.readers.get(k, 0) < v:
                r.readers[k] = v
        for w in writes:
            if parallel:
                w.extra_w.append(ev)
            else:
                w.last_w = ev
                w.extra_w = []
            w.readers = {}

    def op(self, e, fn, reads=(), writes=(), inc=True):
        self._deps(e, reads, writes)
        ev = (e, self.cnt[e] + 1)
        if inc:
            self.cnt[e] += 1
            self.prog[e].append(("op", _capture(fn), e, 1))
        else:
            self.prog[e].append(("op", _capture(fn), None, 0))
        self._commit(ev, reads, writes)
        self.n_ops += 1

    def dma(self, q, fn, reads=(), writes=(), parallel=False):
        sems = self.dma_sems[q]
        k = sems[self.dma_rr[q] % len(sems)]
        self.dma_rr[q] += 1
        if self.dma_tot[k] > 0:
            self._wait(q, (k, self.dma_tot[k]))
        self._deps(q, reads, writes, parallel)
        self.dma_tot[k] += 16
        ev = (k, self.dma_tot[k])
        self.prog[q].append(("op", _capture(fn), k, 16))
        self._commit(ev, reads, writes, parallel)
        return ev

    def finish(self, e="sp"):
        for k, v in self.dma_tot.items():
            if v > 0:
                self._wait(e, (k, v))
        for k in ENGS:
            if k != e and self.cnt[k] > 0:
                self._wait(e, (k, self.cnt[k]))

    def replay(self, nc, block, sems):
        handles = {"pe": "tensor", "act": "scalar", "dve": "vector", "pool": "gpsimd", "sp": "sync"}

        def make(e):
            def body(eng):
                for item in self.prog[e]:
                    if item[0] == "wait":
                        eng.wait_ge(sems[item[1]], item[2])
                    else:
                        name, a, k = item[1]
                        ins = getattr(eng, name)(*a, **k)
                        if item[2] is not None:
                            ins.then_inc(sems[item[2]], item[3])
            return body

        for e in ENGS:
            getattr(block, handles[e])(make(e))


class _Rec:
    def __init__(self):
        self.call = None

    def __getattr__(self, name):
        def f(*a, **k):
            self.call = (name, a, k)
            return self
        return f


def _capture(fn):
    r = _Rec()
    fn(r)
    assert r.call is not None
    return r.call


class RR:
    def __init__(self, tiles, name):
        self.tiles = tiles
        self.regs = [Region(f"{name}{i}") for i in range(len(tiles))]
        self.i = 0

    def next(self):
        j = self.i % len(self.tiles)
        self.i += 1
        return self.tiles[j], self.regs[j]


def alibi_tables():
    slopes = np.array([2.0 ** (-8.0 * (i + 1) / 16) for i in range(16)], dtype=np.float64)
    s_idx = np.arange(128)[:, None]
    q_idx = np.arange(128)[None, :]
    E = np.zeros((128, 4, 2, 2, 2, 128), dtype=np.float32)
    for g in range(4):
        for i in range(2):
            for cc in range(2):
                h = 4 * g + 2 * cc + i
                d1 = (q_idx - s_idx).astype(np.float64)
                E[:, g, i, 1, cc, :] = np.where(d1 >= 0, np.exp(-slopes[h] * d1), 0.0)
                d0 = (q_idx + 128 - s_idx).astype(np.float64)
                E[:, g, i, 0, cc, :] = np.where(d0 < 128, np.exp(-slopes[h] * d0), 0.0)
    return E.reshape(128, 4096)


class _Stop(Exception):
    pass


def build_nc(n_st=4, dbg=False, upto=None):
    nc = bass.Bass("TRN2", target_bir_lowering=False)

    def din(name, shape):
        return nc.dram_tensor(name, list(shape), F32, kind="ExternalInput").ap()

    x = din("x", [S, D])
    c_fm = din("c_fm", [128, 8])
    w_ada = din("w_ada", [D, 6 * D])
    b_ada_fm = din("b_ada_fm", [128, 48])
    b_ada_row = din("b_ada_row", [1, 6 * D])
    n1_fm = din("n1_fm", [128, 8])
    n2_fm = din("n2_fm", [128, 8])
    w_in = din("w_in", [D, 5632])
    bgate_fm = din("bgate_fm", [128, 16])
    qg_fm = din("qg_fm", [128, 1])
    kg_fm = din("kg_fm", [128, 1])
    sinks_row = din("sinks_row", [1, 16])
    gg_fm = din("gg_fm", [128, 8])
    gb_rows = din("gb_rows", [8, 128])
    wsT = din("wsT", [128, 1024])
    bsp_row = din("bsp_row", [1, 1024])
    w_oa = din("w_oa", [D, D])
    w_og = din("w_og", [D, D])
    w_out = din("w_out", [D, D])
    wr = din("wr", [D, 20])
    br_row = din("br_row", [1, 20])
    w_eg = din("w_eg", [16, D, 512])
    w_eu = din("w_eu", [16, D, 512])
    w_ed = din("w_ed", [16, 512, D])
    k_ident = din("k_ident", [128, 128])
    k_etab = din("k_etab", [128, 4096])
    k_maskT = din("k_maskT", [128, 128])
    k_bones = din("k_bones", [128, 128])
    k_sel = din("k_sel", [16, 2048])
    k_ones = din("k_ones", [1, 128])
    out = nc.dram_tensor("out", [S, D], F32, kind="ExternalOutput").ap()

    K = Sync()
    es = ExitStack()

    def sb(name, shape, dt):
        return es.enter_context(nc.sbuf_tensor(name, list(shape), dt))

    BB = sb("BB", [128, 16384], BF16)
    B1 = BB[:, 0:8192].rearrange("p (k t) -> p k t", k=8)
    B2 = BB[:, 8192:16384].rearrange("p (k t) -> p k t", k=8)
    acc = BB[:].bitcast(F32).rearrange("p (b d) -> p b d", b=8)
    B3 = sb("B3", [128, 8, 1024], BF16)
    hT = sb("hT", [128, 8, 1024], BF16)
    slots = [sb(f"slot{i}", [128, 4096], BF16) for i in range(NSLOT)]
    kT = sb("kT", [128, 4, 1152], BF16)
    vaug = sb("vaug", [128, 9, 4, 72], BF16)
    etab = sb("etab", [128, 4096], BF16)
    R2 = sb("R2", [128, 8, 128], F32)
    g1b = sb("g1b", [128, 1024], BF16)
    g2b = sb("g2b", [128, 1024], BF16)
    WmT = sb("WmT", [128, 8, 128], BF16)
    ident_f = sb("ident_f", [128, 128], F32)
    ident_b = sb("ident_b", [128, 128], BF16)
    bones = sb("bones", [128, 128], BF16)
    selb = sb("selb", [16, 16, 128], BF16)
    wr_sb = sb("wr_sb", [128, 8, 20], F32)
    br_b = sb("br_b", [128, 20], F32)
    esink = sb("esink", [128, 16], F32)
    combT = sb("combT", [16, 1024], BF16)
    cst = sb("cst", [128, 96], F32)
    modfm = sb("modfm", [128, 48], F32)
    bfm = sb("bfm", [128, 48], F32)
    c_bf = sb("c_bf", [128, 8], BF16)
    c_rep = sb("c_rep", [128, 8, 128], BF16)
    ones_bf = sb("ones_bf", [128, 1], BF16)
    small = sb("small", [128, 128], F32)
    rt_tile = sb("rt_tile", [128, 816], F32)
    r_rt = Region("rt")
    A1 = cst[:, 0:8]; S1 = cst[:, 8:16]; A2 = cst[:, 16:24]; S2 = cst[:, 24:32]
    N1 = cst[:, 32:40]; N2 = cst[:, 40:48]; QG = cst[:, 48:49]; KG = cst[:, 49:50]
    GG = cst[:, 50:58]; BG = cst[:, 58:74]; CF = cst[:, 74:82]; CA = cst[:, 82:90]

    f32a = RR([sb(f"f32a{i}", [128, 1024], F32) for i in range(3)], "f32a")
    f32h = RR([sb(f"f32h{i}", [128, 512], F32) for i in range(5)], "f32h")
    bfh = RR([sb(f"bfh{i}", [128, 512], BF16) for i in range(6)], "bfh")
    ptp = RR([sb(f"pt{i}", [128, 512], BF16) for i in range(6)], "pt")
    xsb = RR([sb(f"xsb{i}", [128, 1024], BF16) for i in range(2)], "xsb")
    ytm = RR([sb(f"ytm{i}", [128, 1024], BF16) for i in range(1)], "ytm")
    hidp = RR([sb(f"hid{i}", [128, 4, 512], BF16) for i in range(2)], "hid")

    PS = es.enter_context(nc.psum_tensor("PS", [128, 4096], F32))
    bank_r = [Region(f"bank{i}") for i in range(8)]
    pcur = [0]

    def nb(n=1):
        c = pcur[0]
        if c % n:
            c += n - (c % n)
        c %= 8
        pcur[0] = c + n
        return c, bank_r[c:c + n]

    hcur = [0]

    def nbhi():
        c = 2 + hcur[0] % 6
        hcur[0] += 1
        return c, bank_r[c:c + 1]

    def bank(i, w=512, off=0):
        return PS[:, i * 512 + off: i * 512 + off + w]

    def bank_bf(i):
        return PS[:, i * 512:(i + 1) * 512].bitcast(BF16)

    r_B1 = Region("B1"); r_B2 = Region("B2"); r_B3 = Region("B3")
    r_hT = [Region(f"hT{i}") for i in range(NBLK)]
    r_acc = [Region(f"acc{i}") for i in range(NBLK)]
    r_slot = [Region(f"slot{i}") for i in range(NSLOT)]
    r_kT = Region("kT"); r_v = Region("vaug")
    r_const = Region("const")
    r_small = Region("small"); r_comb = Region("combT")
    r_sm = [Region(f"sm{i}") for i in range(32)]
    W_B1 = [r_B1] + r_acc[0:4]
    W_B2 = [r_B2] + r_acc[4:8]

    scur = [0]

    def load_unit(src_ap, width=4096, view=None):
        i = scur[0] % NSLOT
        scur[0] += 1
        dst = s8(slots[i]) if view is None else view(slots[i])
        K.dma("pool", lambda e, d=dst, s=src_ap: e.dma_start(out=d, in_=s), reads=[], writes=[r_slot[i]])
        return slots[i], r_slot[i]

    def wview(W, c0, w):
        return W[:, c0:c0 + w].rearrange("(kc p) f -> p kc f", p=128)

    def s8(slot, w=512):
        return slot[:, 0:8 * w].rearrange("p (k f) -> p k f", k=8)

    def dump(name, ap2d, regs, dt):
        if not dbg:
            return
        t = nc.dram_tensor("dbg_" + name, list(ap2d.shape), dt, kind="ExternalOutput").ap()
        K.dma("sp", lambda e: e.dma_start(out=t[:, :], in_=ap2d), reads=list(regs), writes=[])

    def ld(dst, src, q="sp", regs=(r_const,)):
        K.dma(q, lambda e, d=dst, s=src: e.dma_start(out=d, in_=s), reads=[], writes=list(regs), parallel=True)

    r_c0 = Region("c_in")
    ld(CF, c_fm[:, :], regs=(r_c0,))
    ld(bfm[:, 0:48], b_ada_fm[:, :], regs=(r_const,))
    ld(N1, n1_fm[:, :], regs=(r_const,))
    ld(ident_b[:], k_ident[:, :], q="pool", regs=(r_const,))
    scur[0] = 0
    i_first = scur[0] % NSLOT
    K.dma("pool", lambda e: e.dma_start(out=s8(slots[i_first]), in_=wview(w_ada, 0, 512)), reads=[r_c0, r_const], writes=[r_slot[i_first]])
    scur[0] += 1
    ada_pre = [(slots[i_first], r_slot[i_first])] + [load_unit(wview(w_ada, 512 * u, 512)) for u in range(1, 4)]
    ld(N2, n2_fm[:, :]); ld(QG, qg_fm[:, :]); ld(KG, kg_fm[:, :])
    ld(GG, gg_fm[:, :]); ld(BG, bgate_fm[:, :])
    ld(ident_f[:], k_ident[:, :])
    ld(wr_sb[:], wr.rearrange("(kc p) j -> p kc j", p=128))
    ld(br_b[:], br_row.partition_broadcast(128))
    ld(esink[:], sinks_row.partition_broadcast(128))
    wsf, wsf_r = f32a.next()
    r2l_t, r2l_r = f32a.next()
    r2r_t, r2r_r = f32a.next()
    r2l = r2l_t[0:2, :].rearrange("p (g t) -> p g t", g=8)
    r2r = r2r_t[0:2, :]
    ld(r2l[0:1, :, :], gb_rows.rearrange("(o g) t -> o g t", o=1), regs=(r2l_r,))
    for g in range(8):
        ld(r2l[1:2, g, :], k_ones[0:1, :], regs=(r2l_r,))
    ld(r2r[1:2, :], bsp_row[0:1, :], regs=(r2r_r,))
    ld(wsf[:], wsT[:, :], regs=(wsf_r,))
    mkt, mkt_r = f32h.next()
    ld(mkt[:, 0:128], k_maskT[:, :], regs=(mkt_r,))
    ld(bones[:], k_bones[:, :], q="pool")
    ld(ones_bf[:], k_ones[0:1, :].rearrange("o p -> p o"), q="pool")
    ld(etab[:, 0:2048], k_etab[:, 0:2048], q="pool")
    ld(etab[:, 2048:4096], k_etab[:, 2048:4096], q="pool")
    ld(selb[:].rearrange("k e m -> k (e m)"), k_sel[:, :], q="pool")

    r_c = Region("c_act")
    K.op("act", lambda e: e.activation(out=CA, in_=CF, func=AF.Silu), reads=[r_c0], writes=[r_c0])
    K.op("dve", lambda e: e.tensor_copy(out=c_bf[:], in_=CA), reads=[r_c0], writes=[r_c])
    K.op("dve", lambda e: e.tensor_copy(out=c_rep[:], in_=CA.unsqueeze(2).to_broadcast([128, 8, 128])),
         reads=[r_c0], writes=[r_c])
    K.op("act", lambda e: e.activation(out=esink[:], in_=esink[:], func=AF.Exp), reads=[r_const], writes=[r_const])
    K.op("dve", lambda e: e.tensor_tensor(out=WmT[:], in0=wsf[:].rearrange("p (g t) -> p g t", g=8),
                                          in1=mkt[:, 0:128].unsqueeze(1).to_broadcast([128, 8, 128]), op=ALU.mult),
         reads=[wsf_r, mkt_r], writes=[r_const])
    K.op("dve", lambda e: e.memset(vaug[:], 1.0), reads=[], writes=[r_v])
    K.op("dve", lambda e: e.memset(small[:, 120:121], -0.5), reads=[], writes=[r_const])
    K.op("dve", lambda e: e.memset(kT[:], 0.0), reads=[], writes=[r_kT])

    b0, br_ = nb(2)
    for hh in range(2):
        K.op("pe", lambda e, hh=hh: e.matmul(PS[0:1, (b0 + hh) * 512:(b0 + hh + 1) * 512], ones_bf[:, 0:1],
                                            WmT[:, 4 * hh:4 * hh + 4, :], start=True, stop=True),
             reads=[r_const], writes=[br_[hh]])
    K.op("act", lambda e: e.activation(out=r2r[0:1, :], in_=PS[0:1, b0 * 512:(b0 + 2) * 512], func=AF.Copy),
         reads=br_, writes=[r2r_r])
    b0, br_ = nb(2)
    for g in range(8):
        K.op("pe", lambda e, g=g: e.matmul(PS[:, b0 * 512 + g * 128: b0 * 512 + (g + 1) * 128], r2l[0:2, g, :],
                                          r2r[0:2, g * 128:(g + 1) * 128], start=True, stop=True),
             reads=[r_const, r2l_r, r2r_r], writes=[br_[g // 4]], inc=(g % 4 == 3))
    K.op("act", lambda e: e.activation(out=R2[:].rearrange("p g t -> p (g t)"), in_=PS[:, b0 * 512:(b0 + 2) * 512],
                                       func=AF.Copy), reads=br_, writes=[r_const])

    r_g1 = Region("g1b"); r_a2 = Region("a2s2"); r_g2 = Region("g2b"); r_mod = Region("modfm")

    def ada_fm(units, lo_, hi_, pre=None):
        mb, mbr = nb(1)
        for n_, u in enumerate(units):
            slot, sr = pre[n_] if pre is not None else load_unit(wview(w_ada, 512 * u, 512))
            w8 = s8(slot)
            for fl in range(4):
                j = 4 * u + fl - lo_
                for kc in range(8):
                    K.op("pe", lambda e, kc=kc: e.matmul(
                        bank(mb, 1, j), w8[:, kc, fl * 128:(fl + 1) * 128], c_bf[:, kc:kc + 1],
                        start=(kc == 0), stop=(kc == 7)),
                         reads=[sr, r_c], writes=mbr, inc=(kc == 7 and fl == 3))
        K.op("dve", lambda e: e.tensor_tensor(out=modfm[:, lo_:hi_], in0=bank(mb, hi_ - lo_), in1=bfm[:, lo_:hi_], op=ALU.add),
             reads=mbr + [r_const], writes=[r_mod])

    def ada_rows(units, dst, dst_r):
        for n_, u in enumerate(units):
            slot, sr = load_unit(wview(w_ada, 512 * u, 512))
            w8 = s8(slot)
            gb, gbr = nb(1)
            for kc in range(8):
                K.op("pe", lambda e, kc=kc: e.matmul(bank(gb), c_rep[:, kc, :], w8[:, kc, :], start=(kc == 0), stop=(kc == 7)),
                     reads=[sr, r_c], writes=gbr, inc=(kc == 7))
            bt, bt_r = f32h.next()
            ld(bt[:], b_ada_row[0:1, 512 * u:512 * (u + 1)].partition_broadcast(128), regs=(bt_r,))
            K.op("dve", lambda e: e.tensor_tensor(out=dst[:, 512 * n_:512 * (n_ + 1)], in0=bank(gb), in1=bt[:], op=ALU.add),
                 reads=gbr + [bt_r], writes=[dst_r])

    ada_fm([0, 1, 2, 3], 0, 16, pre=ada_pre)
    K.op("dve", lambda e: e.scalar_tensor_tensor(out=A1, in0=modfm[:, 8:16], scalar=1.0, in1=N1, op0=ALU.add, op1=ALU.mult),
         reads=[r_const, r_mod], writes=[r_const])
    K.op("dve", lambda e: e.tensor_copy(out=S1, in_=modfm[:, 0:8]), reads=[r_mod], writes=[r_const])

    def ada_late_1():
        ada_rows([4, 5], g1b, r_g1)

    def ada_late_2():
        ada_fm([6, 7, 8, 9], 24, 40)
        K.op("dve", lambda e: e.scalar_tensor_tensor(out=A2, in0=modfm[:, 32:40], scalar=1.0, in1=N2, op0=ALU.add, op1=ALU.mult),
             reads=[r_const, r_mod], writes=[r_a2])
        K.op("dve", lambda e: e.tensor_copy(out=S2, in_=modfm[:, 24:32]), reads=[r_mod], writes=[r_a2])
        ada_rows([10, 11], g2b, r_g2)

    dump("modfm", modfm[:], [r_mod], F32)
    dump("R2", R2[:].rearrange("p g t -> p (g t)"), [r_const], F32)
    dump("cst", cst[:], [r_const], F32)

    def stop_if(name):
        if upto == name:
            raise _Stop()

    def bc8(v):
        return v.unsqueeze(2).to_broadcast([128, 8, 128])

    def rstd_pool(src_ap, src_regs, col, junk_ap, junk_r):
        ss = small[:, col:col + 1]
        rr = r_sm[col]
        K.op("act", lambda e: e.activation(out=junk_ap, in_=src_ap, func=AF.Square, accum_out=ss),
             reads=list(src_regs), writes=[junk_r, rr])
        K.op("dve", lambda e: e.tensor_scalar(out=ss, in0=ss, scalar1=1.0 / D, scalar2=EPS, op0=ALU.mult, op1=ALU.add),
             reads=[rr], writes=[rr])
        K.op("pool", lambda e: e.tensor_tensor(out=ss, in0=ss, in1=small[:, 120:121], op=ALU.pow),
             reads=[rr, r_const], writes=[rr])
        return ss, rr

    def rstd_of(src_ap, src_regs, col, junk_ap, junk_r):
        ss = small[:, col:col + 1]
        rr = r_sm[col]
        K.op("act", lambda e: e.activation(out=junk_ap, in_=src_ap, func=AF.Square, accum_out=ss),
             reads=list(src_regs), writes=[junk_r, rr])
        K.op("act", lambda e: e.activation(out=ss, in_=ss, func=AF.Ln, scale=1.0 / D, bias=EPS),
             reads=[rr], writes=[rr])
        K.op("act", lambda e: e.activation(out=ss, in_=ss, func=AF.Exp, scale=-0.5),
             reads=[rr], writes=[rr])
        return ss, rr

    def stage_A_act(st, blk):
        t0 = st * STT
        xin, xin_r = f32a.next()
        K.dma("sp", lambda e: e.dma_start(out=xin[:], in_=x[t0 + blk * 128: t0 + (blk + 1) * 128, :]), reads=[], writes=[xin_r])
        xs, xs_r = xsb.next()
        rs, rs_r = rstd_pool(xin[:], [xin_r], blk % 8, xs[:], xs_r)
        K.op("act", lambda e: e.activation(out=xs[:], in_=xin[:], func=AF.Copy, scale=rs), reads=[xin_r, rs_r], writes=[xs_r])
        return xs, xs_r

    def stage_A_pe(st, blk, xs, xs_r):
        pb, pbr = nb(1)
        for kc in range(8):
            K.op("pe", lambda e, kc=kc: e.transpose(out=bank_bf(pb)[:, kc * 128:(kc + 1) * 128],
                                                    in_=xs[:, kc * 128:(kc + 1) * 128], identity=ident_b[:]),
                 reads=[xs_r, r_const], writes=pbr, inc=(kc == 7))
        for kc in range(8):
            K.op("dve", lambda e, kc=kc: e.tensor_scalar(
                out=hT[:, kc, blk * 128:(blk + 1) * 128], in0=bank_bf(pb)[:, kc * 128:(kc + 1) * 128],
                scalar1=A1[:, kc:kc + 1], scalar2=S1[:, kc:kc + 1], op0=ALU.mult, op1=ALU.add),
                 reads=pbr + [r_const], writes=[r_hT[blk]])

    def stage_A(st):
        prev = None
        for blk in range(NBLK):
            cur = stage_A_act(st, blk)
            if prev is not None:
                stage_A_pe(st, blk - 1, *prev)
            prev = cur
        stage_A_pe(st, NBLK - 1, *prev)

    def body(st):
        t0 = st * STT
        stop_if("setup")
        if st == 0 or NOHOIST:
            stage_A(st)
        stop_if("A")
        if st == 0:
            dump("hT", hT[:].rearrange("p k t -> p (k t)"), r_hT, BF16)
        for j in range(2):
            slot, sr = load_unit(wview(w_in, 1536 + 512 * j, 512))
            w8 = s8(slot)
            for c in range(2):
                for fl in range(4):
                    pb, pbr = nb(1)
                    for kc in range(8):
                        K.op("pe", lambda e, kc=kc, w8=w8, fl=fl, c=c, pb=pb: e.matmul(
                            bank(pb), w8[:, kc, fl * 128:(fl + 1) * 128], hT[:, kc, c * 512:(c + 1) * 512],
                            start=(kc == 0), stop=(kc == 7)),
                             reads=[sr] + r_hT[4 * c:4 * c + 4], writes=pbr, inc=(kc == 7))
                    K.op("act", lambda e, pb=pb, j=j, fl=fl, c=c: e.activation(
                        out=B1[:, 4 * j + fl, c * 512:(c + 1) * 512], in_=bank(pb), func=AF.Gelu),
                         reads=pbr, writes=W_B1)
        vs = [load_unit(wview(w_in, 2560 + 512 * j, 512)) for j in range(2)]
        qs_pre = [load_unit(wview(w_in, 512 * j, 512)) for j in range(2)]
        i_k = scur[0] % NSLOT
        scur[0] += 1
        kslot, ksr = slots[i_k], r_slot[i_k]
        for dup in range(2):
            for kv in range(4):
                c0 = kv * 128 + dup * 64
                dstv = s8(kslot)[:, :, c0:c0 + 64]
                srcv = wview(w_in, 1024 + kv * 64, 64)
                K.dma("pool", lambda e, d=dstv, s=srcv: e.dma_start(out=d, in_=s), reads=[], writes=[ksr], parallel=(dup + kv > 0))
        v_pre = load_unit(wview(w_in, 1280, 256), width=2048, view=lambda s_: s_[:, 0:2048].rearrange("p (k f) -> p k f", k=8))
        def vg_block(blk):
            gv, gv_r = f32a.next()
            for j in range(2):
                pb, pbr = nb(1)
                w8 = s8(vs[j][0])
                for kc in range(8):
                    K.op("pe", lambda e, kc=kc: e.matmul(
                        bank(pb), hT[:, kc, blk * 128:(blk + 1) * 128], w8[:, kc, :], start=(kc == 0), stop=(kc == 7)),
                         reads=[vs[j][1], r_hT[blk]], writes=pbr, inc=(kc == 7))
                K.op("act", lambda e: e.activation(out=gv[:, 512 * j:512 * (j + 1)], in_=bank(pb), func=AF.Gelu),
                     reads=pbr, writes=[gv_r])
            so_ = 16 + 16 * (blk % 4)
            lr = r_sm[16 + blk % 4]
            for j in range(2):
                K.op("dve", lambda e, j=j: e.bn_stats(out=small[:, so_ + 6 * j:so_ + 6 + 6 * j], in_=gv[:, 512 * j:512 * (j + 1)]),
                     reads=[gv_r], writes=[lr])
            K.op("dve", lambda e: e.bn_aggr(out=small[:, so_ + 12:so_ + 14], in_=small[:, so_:so_ + 12]), reads=[lr], writes=[lr])
            K.op("dve", lambda e: e.tensor_scalar(out=small[:, so_ + 14:so_ + 15], in0=small[:, so_ + 13:so_ + 14],
                                                  scalar1=EPS, scalar2=None, op0=ALU.add), reads=[lr], writes=[lr])
            K.op("pool", lambda e: e.tensor_tensor(out=small[:, so_ + 15:so_ + 16], in0=small[:, so_ + 14:so_ + 15],
                                                   in1=small[:, 120:121], op=ALU.pow), reads=[lr, r_const], writes=[lr])
            K.op("dve", lambda e: e.tensor_scalar(
                out=B2[:, blk, :], in0=gv[:], scalar1=small[:, so_ + 12:so_ + 13], scalar2=small[:, so_ + 15:so_ + 16],
                op0=ALU.subtract, op1=ALU.mult), reads=[gv_r, lr], writes=W_B2)
            return gv, gv_r

        def sp_block(blk, tmp, tmp_r):
            b0, b0r = nb(2)
            for g in range(8):
                K.op("pe", lambda e, g=g: e.matmul(
                    PS[:, b0 * 512 + g * 128: b0 * 512 + (g + 1) * 128], B2[:, blk, g * 128:(g + 1) * 128], WmT[:, g, :],
                    start=True, stop=True), reads=[r_B2, r_const], writes=[b0r[g // 4]], inc=(g % 4 == 3))
            t3 = tmp[:].rearrange("p (g t) -> p g t", g=8)
            K.op("dve", lambda e: e.tensor_tensor(
                out=t3, in0=PS[:, b0 * 512:(b0 + 2) * 512].rearrange("p (g t) -> p g t", g=8), in1=bc8(GG), op=ALU.mult),
                 reads=b0r + [r_const], writes=[tmp_r])
            K.op("dve", lambda e: e.tensor_tensor(out=t3, in0=t3, in1=R2[:], op=ALU.add), reads=[tmp_r, r_const], writes=[tmp_r])
            K.op("dve", lambda e: e.tensor_tensor(
                out=B1[:, :, blk * 128:(blk + 1) * 128], in0=t3, in1=B1[:, :, blk * 128:(blk + 1) * 128], op=ALU.mult),
                 reads=[tmp_r, r_B1], writes=W_B1)

        LOOK = 2
        gvs = {}
        for blk in range(NBLK + LOOK):
            if blk < NBLK:
                gvs[blk] = vg_block(blk)
            if blk - LOOK >= 0:
                sp_block(blk - LOOK, *gvs.pop(blk - LOOK))
        stop_if("B1")

        if st == 0:
            dump("ygT_", BB[:, 0:8192], [r_B1], BF16) if False else None
        stop_if("B")
        if st == 0:
            dump("ygT", BB[:, 0:8192], [r_B1], BF16)
        def qk_norm(pb, pbr, gain, dst_ap, dst_regs):
            sq, sq_r = bfh.next()
            K.op("act", lambda e: e.activation(out=sq[:], in_=bank(pb), func=AF.Square), reads=pbr, writes=[sq_r])
            p2, p2r = nb(1)
            K.op("pe", lambda e: e.matmul(bank(p2), bones[:], sq[:], start=True, stop=True),
                 reads=[sq_r, r_const], writes=p2r)
            rq, rq_r = f32h.next()
            K.op("act", lambda e: e.activation(out=rq[:], in_=bank(p2), func=AF.Ln, bias=64.0 * EPS), reads=p2r, writes=[rq_r])
            K.op("act", lambda e: e.activation(out=rq[:], in_=rq[:], func=AF.Exp, scale=-0.5), reads=[rq_r], writes=[rq_r])
            K.op("dve", lambda e: e.scalar_tensor_tensor(out=dst_ap, in0=bank(pb), scalar=gain, in1=rq[:],
                                                         op0=ALU.mult, op1=ALU.mult),
                 reads=pbr + [rq_r, r_const], writes=dst_regs)

        qk_pending = []

        def qk_task(w8, col, c, sr, gain, dst, dst_regs):
            pb, pbr = nb(1)
            for kc in range(8):
                K.op("pe", lambda e, kc=kc: e.matmul(
                    bank(pb), w8[:, kc, col:col + 128], hT[:, kc, c * 512:(c + 1) * 512],
                    start=(kc == 0), stop=(kc == 7)),
                     reads=[sr] + r_hT[4 * c:4 * c + 4], writes=pbr, inc=(kc == 7))
            if qk_pending:
                qk_norm(*qk_pending.pop())
            qk_pending.append((pb, pbr, gain, dst, dst_regs))

        for j in range(2):
            slot, sr = qs_pre[j]
            w8 = s8(slot)
            for c in range(2):
                for fl in range(4):
                    qk_task(w8, fl * 128, c, sr, QG, B3[:, 4 * j + fl, c * 512:(c + 1) * 512], [r_B3])
        w8 = s8(kslot)
        for c in range(2):
            for kv in range(4):
                qk_task(w8, kv * 128, c, ksr, KG, kT[:, kv, 128 + c * 512:128 + (c + 1) * 512], [r_kT])
        qk_norm(*qk_pending.pop())
        slot, sr = v_pre
        wv8 = slot[:, 0:2048].rearrange("p (k f) -> p k f", k=8)
        for blk in range(NBLK):
            pb, pbr = nb(1)
            for kc in range(8):
                K.op("pe", lambda e, kc=kc, blk=blk, pb=pb: e.matmul(
                    bank(pb, 256), hT[:, kc, blk * 128:(blk + 1) * 128], wv8[:, kc, :], start=(kc == 0), stop=(kc == 7)),
                     reads=[sr, r_hT[blk]], writes=pbr, inc=(kc == 7))
            K.op("act", lambda e, blk=blk, pb=pb: e.activation(
                out=vaug[:, 1 + blk, :, 0:64], in_=bank(pb, 256).rearrange("p (v d) -> p v d", v=4), func=AF.Copy),
                 reads=pbr, writes=[r_v])
        stop_if("C1")
        if st == 0:
            dump("qT", B3[:].rearrange("p k t -> p (k t)"), [r_B3], BF16)
            dump("kT", kT[:].rearrange("p k t -> p (k t)"), [r_kT], BF16)
            dump("vaug", vaug[:].rearrange("p b v d -> p (b v d)"), [r_v], BF16)
        def att_qk(blk, g, js):
            pts = {}
            lo = 256 if len(js) == 1 else 0
            for i in range(2):
                pb, pbr = nbhi()
                for jj in js:
                    koff = blk * 128 + jj * 128
                    K.op("pe", lambda e, jj=jj, koff=koff: e.matmul(
                        bank(pb, 256, 256 * jj).rearrange("p (c q) -> p c q", c=2),
                        kT[64 * i:64 * i + 64, g, koff:koff + 128],
                        B3[64 * i:64 * i + 64, 2 * g:2 * g + 2, blk * 128:(blk + 1) * 128],
                        start=True, stop=True), reads=[r_kT, r_B3], writes=pbr, inc=(jj == 1))
                pe_, pe_r = bfh.next()
                K.op("act", lambda e: e.activation(out=pe_[:, lo:512], in_=bank(pb, 512 - lo, lo), func=AF.Exp, scale=8.0),
                     reads=pbr, writes=[pe_r])
                pt, pt_r = ptp.next()
                eo = (g * 2 + i) * 512
                K.op("pool" if i == 1 else "dve",
                     lambda e: e.tensor_tensor(out=pt[:, lo:512], in0=pe_[:, lo:512], in1=etab[:, eo + lo:eo + 512], op=ALU.mult),
                     reads=[pe_r, r_const], writes=[pt_r])
                pts[i] = (pt, pt_r)
            return pts

        def att_pv(blk, g, js, pts, yps, ybr):
            for hl in range(4):
                h = 4 * g + hl
                i = h % 2
                cc = (h % 4) // 2
                pt, pt_r = pts[i]
                for n_, jj in enumerate(js):
                    col = jj * 256 + cc * 128
                    K.op("pe", lambda e, n_=n_, jj=jj, col=col: e.matmul(
                        yps[:, 4 * (g % 2) + hl, 0:65], pt[:, col:col + 128], vaug[:, blk + jj, g, 0:65],
                        start=(n_ == 0), stop=(n_ == len(js) - 1)),
                         reads=[pt_r, r_v], writes=[ybr[g % 2]], inc=(n_ == len(js) - 1 and hl == 3))

        def att_norm(blk, g, yps, ybr, yt, yt_r):
            k4 = (4 * blk + g) % 4
            den = small[:, 80 + 4 * k4:84 + 4 * k4]
            dr = r_sm[24 + k4]
            yg = yps[:, 4 * (g % 2):4 * (g % 2) + 4, :]
            K.op("dve", lambda e: e.tensor_tensor(out=den.unsqueeze(2), in0=yg[:, :, 64:65],
                                                  in1=esink[:, 4 * g:4 * g + 4].unsqueeze(2), op=ALU.add),
                 reads=[ybr[g % 2], r_const], writes=[dr])
            K.op("dve", lambda e: e.reciprocal(out=den, in_=den), reads=[dr], writes=[dr])
            K.op("dve", lambda e: e.tensor_tensor(
                out=yt[:, 256 * g:256 * (g + 1)].rearrange("p (h d) -> p h d", h=4), in0=yg[:, :, 0:64],
                in1=den.unsqueeze(2).to_broadcast([128, 4, 64]), op=ALU.mult),
                 reads=[ybr[g % 2], dr], writes=[yt_r])

        def att_fin(blk, yt, yt_r):
            pb, pbr = nbhi()
            for kc in range(8):
                K.op("pe", lambda e, kc=kc: e.transpose(out=bank_bf(pb)[:, kc * 128:(kc + 1) * 128],
                                                        in_=yt[:, kc * 128:(kc + 1) * 128], identity=ident_b[:]),
                     reads=[yt_r, r_const], writes=pbr, inc=(kc == 7))
            K.op("act", lambda e: e.activation(
                out=B2[:, :, blk * 128:(blk + 1) * 128], in_=bank_bf(pb).rearrange("p (k t) -> p k t", k=8), func=AF.Copy),
                 reads=pbr, writes=W_B2)

        pre_d = [load_unit(wview(w_oa, 0, 512)), load_unit(wview(w_in, 3584, 512))]
        ybr = bank_r[0:2]
        yps = PS[:, 0:1024].rearrange("p (h d) -> p h d", h=8)
        units = [(blk, g) for blk in range(NBLK) for g in range(4)]
        jsof = lambda blk: [1] if (st * NBLK + blk) == 0 else [0, 1]
        ALOOK = 2
        pend = {}
        yts = {}
        for n in range(len(units) + ALOOK):
            if n < len(units):
                b2, g2 = units[n]
                pend[n] = att_qk(b2, g2, jsof(b2))
            m_ = n - ALOOK
            if m_ >= 0:
                blk, g = units[m_]
                if g == 0:
                    yts[blk] = ytm.next()
                att_pv(blk, g, jsof(blk), pend.pop(m_), yps, ybr)
                att_norm(blk, g, yps, ybr, *yts[blk])
                if g == 3:
                    att_fin(blk, *yts.pop(blk))
        stop_if("C")
        if st == 0:
            dump("yaT", BB[:, 8192:16384], [r_B2], BF16)
        K.op("dve", lambda e: e.tensor_copy(out=kT[:, :, 0:128], in_=kT[:, :, 1024:1152]), reads=[r_kT], writes=[r_kT])
        K.op("dve", lambda e: e.tensor_copy(out=vaug[:, 0, :, :], in_=vaug[:, 8, :, :]), reads=[r_v], writes=[r_v])

        if st == 0:
            ada_late_1()
        for j in range(2):
            for br_i, (Wo, src, src_r, gcol) in enumerate(((w_oa, B2, r_B2, 3584), (w_og, B1, r_B1, 4608))):
                if j == 0 and br_i == 0:
                    (so, sor), (sg_, sgr) = pre_d
                else:
                    so, sor = load_unit(wview(Wo, 512 * j, 512))
                    sg_, sgr = load_unit(wview(w_in, gcol + 512 * j, 512))
                wo8 = s8(so); wg8 = s8(sg_)
                for c in range(2):
                    for fl in range(4):
                        fch = 4 * j + fl
                        pa, par = nb(1)
                        for kc in range(8):
                            K.op("pe", lambda e, kc=kc, wo8=wo8, fl=fl, c=c, pa=pa, src=src: e.matmul(
                                bank(pa), wo8[:, kc, fl * 128:(fl + 1) * 128], src[:, kc, c * 512:(c + 1) * 512],
                                start=(kc == 0), stop=(kc == 7)), reads=[sor, src_r], writes=par, inc=(kc == 7))
                        pg, pgr = nb(1)
                        for kc in range(8):
                            K.op("pe", lambda e, kc=kc, wg8=wg8, fl=fl, c=c, pg=pg: e.matmul(
                                bank(pg), wg8[:, kc, fl * 128:(fl + 1) * 128], hT[:, kc, c * 512:(c + 1) * 512],
                                start=(kc == 0), stop=(kc == 7)), reads=[sgr] + r_hT[4 * c:4 * c + 4], writes=pgr, inc=(kc == 7))
                        sg, sg_r = f32h.next()
                        bcol = BG[:, 8 * br_i + fch: 8 * br_i + fch + 1]
                        K.op("act", lambda e, sg=sg, pg=pg, bcol=bcol: e.activation(out=sg[:], in_=bank(pg), func=AF.Sigmoid, bias=bcol),
                             reads=pgr + [r_const], writes=[sg_r])
                        dst = B3[:, fch, c * 512:(c + 1) * 512]
                        if br_i == 0:
                            K.op("dve", lambda e, sg=sg, pa=pa, dst=dst: e.tensor_tensor(out=dst, in0=bank(pa), in1=sg[:], op=ALU.mult),
                                 reads=par + [sg_r], writes=[r_B3])
                        else:
                            t2, t2_r = f32h.next()
                            K.op("dve", lambda e, sg=sg, pa=pa, t2=t2: e.tensor_tensor(out=t2[:], in0=bank(pa), in1=sg[:], op=ALU.mult),
                                 reads=par + [sg_r], writes=[t2_r])
                            K.op("dve", lambda e, t2=t2, dst=dst: e.tensor_tensor(out=dst, in0=t2[:], in1=dst, op=ALU.add),
                                 reads=[t2_r, r_B3], writes=[r_B3])

        stop_if("D")
        if st == 0:
            dump("mergedT", B3[:].rearrange("p k t -> p (k t)"), [r_B3], BF16)
        if st == 0:
            ada_late_2()
        for j in range(2):
            slot, sr = load_unit(wview(w_out, 512 * j, 512))
            w8 = s8(slot)
            K.op("dve", lambda e, w8=w8, j=j: e.tensor_tensor(
                out=w8, in0=w8, in1=g1b[:, 512 * j:512 * (j + 1)].unsqueeze(1).to_broadcast([128, 8, 512]), op=ALU.mult),
                 reads=[sr, r_g1], writes=[sr])
            for blk in range(NBLK):
                xh, xh_r = f32h.next()
                K.dma("sp", lambda e, xh=xh, blk=blk, j=j: e.dma_start(
                    out=xh[:], in_=x[t0 + blk * 128: t0 + (blk + 1) * 128, 512 * j:512 * (j + 1)]), reads=[], writes=[xh_r])
                pb, pbr = nb(1)
                for kc in range(8):
                    K.op("pe", lambda e, kc=kc, w8=w8, blk=blk, pb=pb: e.matmul(
                        bank(pb), B3[:, kc, blk * 128:(blk + 1) * 128], w8[:, kc, :], start=(kc == 0), stop=(kc == 7)),
                         reads=[sr, r_B3], writes=pbr, inc=(kc == 7))
                K.op("dve", lambda e, xh=xh, pb=pb, blk=blk, j=j: e.tensor_tensor(
                    out=acc[:, blk, 512 * j:512 * (j + 1)], in0=bank(pb), in1=xh[:], op=ALU.add),
                     reads=pbr + [xh_r], writes=[r_acc[blk], r_B1 if blk < 4 else r_B2])

        stop_if("E")
        if st == 0:
            dump("xn", BB[:].bitcast(F32), r_acc, F32)
        rt, rt_r = rt_tile, r_rt
        GL = rt[:, 0:32].rearrange("p (b g) -> p b g", b=8)
        EL = rt[:, 32:160]
        def f_head(blk):
            xs32, xs32_r = f32a.next()
            rs, rs_r = rstd_of(acc[:, blk, :], [r_acc[blk]], 8 + blk % 8, xs32[:], xs32_r)
            K.op("act", lambda e: e.activation(out=xs32[:], in_=acc[:, blk, :], func=AF.Copy, scale=rs),
                 reads=[r_acc[blk], rs_r], writes=[xs32_r])
            b0, b0r = nb(2)
            for kc in range(8):
                K.op("pe", lambda e, kc=kc: e.transpose(
                    out=PS[:, b0 * 512 + kc * 128: b0 * 512 + (kc + 1) * 128], in_=xs32[:, kc * 128:(kc + 1) * 128],
                    identity=ident_f[:]), reads=[xs32_r, r_const], writes=[b0r[kc // 4]], inc=(kc % 4 == 3))
            h2f, h2f_r = xs32, xs32_r
            h3 = h2f[:].rearrange("p (k t) -> p k t", k=8)
            K.op("dve", lambda e: e.tensor_tensor(
                out=h3, in0=PS[:, b0 * 512:(b0 + 2) * 512].rearrange("p (k t) -> p k t", k=8), in1=bc8(A2), op=ALU.mult),
                 reads=b0r + [r_a2], writes=[h2f_r])
            K.op("dve", lambda e: e.tensor_tensor(out=h3, in0=h3, in1=bc8(S2), op=ALU.add),
                 reads=[h2f_r, r_a2], writes=[h2f_r])
            return h3, h2f_r

        def f_tail(blk, h3, h2f_r):
            K.op("act", lambda e: e.activation(out=B3[:, :, blk * 128:(blk + 1) * 128], in_=h3, func=AF.Copy),
                 reads=[h2f_r], writes=[r_B3])
            pb, pbr = nb(1)
            for kc in range(8):
                K.op("pe", lambda e, kc=kc: e.matmul(bank(pb, 20), h3[:, kc, :], wr_sb[:, kc, :], start=(kc == 0), stop=(kc == 7)),
                     reads=[h2f_r, r_const], writes=pbr, inc=(kc == 7))
            K.op("dve", lambda e: e.tensor_tensor(out=GL[:, blk, :], in0=bank(pb, 4), in1=br_b[:, 0:4], op=ALU.add),
                 reads=pbr + [r_const], writes=[rt_r])
            K.op("dve", lambda e: e.tensor_tensor(out=EL[:, 16 * blk:16 * blk + 16], in0=bank(pb, 16, 4), in1=br_b[:, 4:20], op=ALU.add),
                 reads=pbr + [r_const], writes=[rt_r])

        prev = None
        for blk in range(NBLK):
            cur = f_head(blk)
            if prev is not None:
                f_tail(blk - 1, *prev)
            prev = cur
        f_tail(NBLK - 1, *prev)
        o = 160

        def v8(n):
            nonlocal o
            a = rt[:, o:o + 8 * n].rearrange("p (b n) -> p b n", b=8)
            o += 8 * n
            return a

        ngm = v8(1); T4 = v8(4); goh = v8(4); gex = v8(4); gsum = v8(1); gw = v8(1)
        TM = v8(16); esel = v8(4); nm1 = v8(1); T5 = v8(4); oh1 = v8(4); e2 = v8(4); nm2 = v8(1); oh2 = v8(4)
        dd = v8(1); ed = v8(1); w1 = v8(1); w2 = v8(1); ta = v8(4); wig = v8(4); comb = v8(16)
        RS = [rt_r]

        def sop(fn, eng="dve"):
            K.op(eng, fn, reads=RS, writes=RS)

        def b4(a):
            return a.to_broadcast([128, 8, 4])

        sop(lambda e: e.tensor_reduce(out=ngm, in_=GL, axis=AX.X, op=ALU.max, negate=True))
        sop(lambda e: e.tensor_tensor(out=T4, in0=GL, in1=b4(ngm), op=ALU.add))
        sop(lambda e: e.tensor_single_scalar(out=goh, in_=T4, scalar=0.0, op=ALU.is_ge))
        sop(lambda e: e.activation(out=gex, in_=T4, func=AF.Exp), eng="act")
        sop(lambda e: e.tensor_reduce(out=gsum, in_=gex, axis=AX.X, op=ALU.add))
        sop(lambda e: e.reciprocal(out=gw, in_=gsum))
        for g in range(4):
            sop(lambda e, g=g: e.tensor_tensor(out=TM[:, :, 4 * g:4 * g + 4],
                                               in0=EL.rearrange("p (b n) -> p b n", b=8)[:, :, 4 * g:4 * g + 4],
                                               in1=b4(goh[:, :, g:g + 1]), op=ALU.mult))
        sop(lambda e: e.tensor_tensor(out=esel, in0=TM[:, :, 0:4], in1=TM[:, :, 4:8], op=ALU.add))
        sop(lambda e: e.tensor_tensor(out=esel, in0=esel, in1=TM[:, :, 8:12], op=ALU.add))
        sop(lambda e: e.tensor_tensor(out=esel, in0=esel, in1=TM[:, :, 12:16], op=ALU.add))
        sop(lambda e: e.tensor_reduce(out=nm1, in_=esel, axis=AX.X, op=ALU.max, negate=True))
        sop(lambda e: e.tensor_tensor(out=T5, in0=esel, in1=b4(nm1), op=ALU.add))
        sop(lambda e: e.tensor_single_scalar(out=oh1, in_=T5, scalar=0.0, op=ALU.is_ge))
        sop(lambda e: e.scalar_tensor_tensor(out=e2, in0=oh1, scalar=-1e30, in1=esel, op0=ALU.mult, op1=ALU.add))
        sop(lambda e: e.tensor_reduce(out=nm2, in_=e2, axis=AX.X, op=ALU.max, negate=True))
        sop(lambda e: e.tensor_tensor(out=T5, in0=e2, in1=b4(nm2), op=ALU.add))
        sop(lambda e: e.tensor_single_scalar(out=oh2, in_=T5, scalar=0.0, op=ALU.is_ge))
        sop(lambda e: e.tensor_tensor(out=dd, in0=nm1, in1=nm2, op=ALU.subtract))
        sop(lambda e: e.activation(out=ed, in_=dd, func=AF.Exp), eng="act")
        sop(lambda e: e.tensor_single_scalar(out=w1, in_=ed, scalar=1.0, op=ALU.add))
        sop(lambda e: e.reciprocal(out=w1, in_=w1))
        sop(lambda e: e.tensor_tensor(out=w1, in0=w1, in1=gw, op=ALU.mult))
        sop(lambda e: e.tensor_tensor(out=w2, in0=w1, in1=ed, op=ALU.mult))
        sop(lambda e: e.tensor_tensor(out=ta, in0=oh1, in1=b4(w1), op=ALU.mult))
        sop(lambda e: e.tensor_tensor(out=wig, in0=oh2, in1=b4(w2), op=ALU.mult))
        sop(lambda e: e.tensor_tensor(out=wig, in0=wig, in1=ta, op=ALU.add))
        for g in range(4):
            sop(lambda e, g=g: e.tensor_tensor(out=comb[:, :, 4 * g:4 * g + 4], in0=wig, in1=b4(goh[:, :, g:g + 1]), op=ALU.mult))
        for blk in range(NBLK):
            pc, pcr = nb(1)
            K.op("pe", lambda e, pc=pc, blk=blk: e.transpose(out=PS[0:16, pc * 512: pc * 512 + 128], in_=comb[:, blk, :],
                                                            identity=ident_f[:]),
                 reads=RS + [r_const], writes=pcr)
            K.op("act", lambda e, pc=pc, blk=blk: e.activation(out=combT[0:16, blk * 128:(blk + 1) * 128],
                                                              in_=PS[0:16, pc * 512: pc * 512 + 128], func=AF.Copy),
                 reads=pcr, writes=[r_comb])

        stop_if("F")
        if st == 0:
            dump("h2T", B3[:].rearrange("p k t -> p (k t)"), [r_B3], BF16)
            dump("combT", combT[:], [r_comb], BF16)
        def moe_gu(ex, c, wg8, wu8, sgr, sur):
            hid, hid_r = hidp.next()
            cb, cb_r = bfh.next()
            for fl in range(4):
                pg, pgr = nb(1)
                for kc in range(8):
                    K.op("pe", lambda e, kc=kc: e.matmul(
                        bank(pg), wg8[:, kc, fl * 128:(fl + 1) * 128], B3[:, kc, c * 512:(c + 1) * 512],
                        start=(kc == 0), stop=(kc == 7)), reads=[sgr, r_B3], writes=pgr, inc=(kc == 7))
                pu, pur = nb(1)
                for kc in range(8):
                    K.op("pe", lambda e, kc=kc: e.matmul(
                        bank(pu), wu8[:, kc, fl * 128:(fl + 1) * 128], B3[:, kc, c * 512:(c + 1) * 512],
                        start=(kc == 0), stop=(kc == 7)), reads=[sur, r_B3], writes=pur, inc=(kc == 7))
                if fl == 0:
                    pcb, pcbr = nb(1)
                    K.op("pe", lambda e: e.matmul(bank(pcb), selb[0:16, ex, :], combT[0:16, c * 512:(c + 1) * 512],
                                                  start=True, stop=True), reads=[r_comb, r_const], writes=pcbr)
                    K.op("act", lambda e: e.activation(out=cb[:], in_=bank(pcb), func=AF.Copy), reads=pcbr, writes=[cb_r])
                sl, sl_r = f32h.next()
                K.op("act", lambda e: e.activation(out=sl[:], in_=bank(pg), func=AF.Silu), reads=pgr, writes=[sl_r])
                K.op("dve", lambda e: e.tensor_tensor(out=sl[:], in0=bank(pu), in1=sl[:], op=ALU.mult),
                     reads=pur + [sl_r], writes=[sl_r])
                K.op("dve", lambda e: e.tensor_tensor(out=hid[:, fl, :], in0=sl[:], in1=cb[:], op=ALU.mult),
                     reads=[sl_r, cb_r], writes=[hid_r])
            return hid, hid_r

        def moe_dn(ex, c, hid, hid_r, wd4, sdr):
            for tt in range(4):
                blk = 4 * c + tt
                for dh in range(2):
                    pd, pdr = nb(1)
                    for fl in range(4):
                        K.op("pe", lambda e, fl=fl: e.matmul(
                            bank(pd), hid[:, fl, tt * 128:(tt + 1) * 128], wd4[:, fl, dh * 512:(dh + 1) * 512],
                            start=(fl == 0), stop=(fl == 3)), reads=[hid_r, sdr], writes=pdr, inc=(fl == 3))
                    K.op("dve", lambda e: e.tensor_tensor(
                        out=acc[:, blk, 512 * dh:512 * (dh + 1)], in0=bank(pd), in1=acc[:, blk, 512 * dh:512 * (dh + 1)], op=ALU.add),
                         reads=pdr + [r_acc[blk]], writes=[r_acc[blk]])
                if ex == 15:
                    K.dma("sp", lambda e: e.dma_start(out=out[t0 + blk * 128: t0 + (blk + 1) * 128, :], in_=acc[:, blk, :]),
                          reads=[r_acc[blk]], writes=[])

        for ex in range(16):
            sg_, sgr = load_unit(w_eg[ex].rearrange("(kc p) f -> p kc f", p=128))
            su_, sur = load_unit(w_eu[ex].rearrange("(kc p) f -> p kc f", p=128))
            sd_, sdr = load_unit(w_ed[ex].rearrange("(fc p) d -> p fc d", p=128),
                                 view=lambda s_: s_[:].rearrange("p (k f) -> p k f", k=4))
            wg8 = s8(sg_); wu8 = s8(su_)
            wd4 = sd_[:].rearrange("p (k f) -> p k f", k=4)
            hoist = (ex >= 8 and st + 1 < n_st and upto is None and not NOHOIST)
            if hoist:
                a_nxt = stage_A_act(st + 1, ex - 8)
            h0 = moe_gu(ex, 0, wg8, wu8, sgr, sur)
            K.op("dve", lambda e: e.tensor_tensor(out=wd4, in0=wd4, in1=g2b[:].unsqueeze(1).to_broadcast([128, 4, 1024]),
                                                  op=ALU.mult), reads=[sdr, r_g2], writes=[sdr])
            h1 = moe_gu(ex, 1, wg8, wu8, sgr, sur)
            moe_dn(ex, 0, h0[0], h0[1], wd4, sdr)
            if hoist:
                stage_A_pe(st + 1, ex - 8, *a_nxt)
            moe_dn(ex, 1, h1[0], h1[1], wd4, sdr)

    try:
        for st_ in range(n_st):
            body(st_)
    except _Stop:
        pass
    K.finish("sp")
    sems = {k: es.enter_context(nc.semaphore(k)) for k in K.sem_names()}
    block = es.enter_context(nc.Block())
    K.replay(nc, block, sems)
    es.close()
    return nc


def host_inputs(inp, b):
    f = lambda a: np.ascontiguousarray(a, dtype=np.float32)
    fm = lambda v: f(np.asarray(v).reshape(-1, 128).T)
    m = {}
    m["x"] = f(inp["x"][b])
    m["c_fm"] = fm(inp["c"][b])
    m["w_ada"] = f(inp["w_ada"][0])
    m["b_ada_fm"] = fm(inp["b_ada"][0])
    m["b_ada_row"] = f(inp["b_ada"][0][None, :])
    m["n1_fm"] = fm(inp["norm1_gain"][0])
    m["n2_fm"] = fm(inp["norm2_gain"][0])
    m["w_in"] = f(inp["w_in"][0])
    m["bgate_fm"] = fm(inp["b_branch_gate"][0])
    m["qg_fm"] = f(np.tile(inp["q_norm_gain"][0], 2)[:, None])
    m["kg_fm"] = f(np.tile(inp["k_norm_gain"][0], 2)[:, None])
    m["sinks_row"] = f(inp["attn_sinks"][0][None, :])
    m["gg_fm"] = fm(inp["gmlp_norm_gain"][0])
    m["gb_rows"] = f(inp["gmlp_norm_bias"][0].reshape(8, 128))
    m["wsT"] = f(np.transpose(inp["gmlp_w_spatial"][0], (2, 0, 1)).reshape(128, 1024))
    m["bsp_row"] = f(inp["gmlp_b_spatial"][0].reshape(1, 1024))
    m["w_oa"] = f(inp["w_o_attn"][0])
    m["w_og"] = f(inp["w_o_gmlp"][0])
    m["w_out"] = f(inp["w_out"][0])
    m["wr"] = f(np.concatenate([inp["w_group_router"][0], inp["w_expert_router"][0]], axis=1))
    m["br_row"] = f(np.concatenate([inp["b_group_router"][0], inp["b_expert_router"][0]])[None, :])
    m["w_eg"] = f(inp["w_expert_gate"][0].reshape(16, 1024, 512))
    m["w_eu"] = f(inp["w_expert_up"][0].reshape(16, 1024, 512))
    m["w_ed"] = f(inp["w_expert_down"][0].reshape(16, 512, 1024))
    return m


def host_consts():
    m = {}
    m["k_ident"] = np.eye(128, dtype=np.float32)
    m["k_etab"] = alibi_tables()
    s_idx = np.arange(128)[:, None]
    t_idx = np.arange(128)[None, :]
    m["k_maskT"] = (s_idx <= t_idx).astype(np.float32)
    bo = np.zeros((128, 128), np.float32)
    bo[:64, :64] = 1.0
    bo[64:, 64:] = 1.0
    m["k_bones"] = bo
    sel = np.zeros((16, 16, 128), np.float32)
    for e_ in range(16):
        sel[e_, e_, :] = 1.0
    m["k_sel"] = sel.reshape(16, 2048)
    m["k_ones"] = np.ones((1, 128), np.float32)
    return m


_NC_CACHE = {}


def kernel(**inputs):
    inp = {k: np.asarray(v) for k, v in inputs.items()}
    if "nc" not in _NC_CACHE:
        _NC_CACHE["nc"] = build_nc()
    nc = _NC_CACHE["nc"]
    consts = host_consts()
    in_maps = []
    for b in range(8):
        m = host_inputs(inp, b)
        m.update(consts)
        in_maps.append(m)
    res = run_bass_kernel_spmd(nc, in_maps, core_ids=list(range(8)))
    return np.stack([np.asarray(r["out"]).reshape(S, D) for r in res.results], axis=0).astype(np.float32)
```
